# Optimizing a Trainium2 kernel written in Bass

```python
import jax, jax.numpy as jnp
from jax import lax
import numpy as np

D_MODEL = 2048
BATCH = 1
SEQ = 8192
DEPTH = 1

N_HEADS = 8
HEAD_DIM = 128
KV_LATENT = 256
IDX_HEADS = 16
IDX_DIM = 64
TOPK_MAX = 256
Q_BLOCK = 128
ATTN_WIDTH = N_HEADS * HEAD_DIM
GMLP_GROUPS = 8
GMLP_GROUP_DIM = 128
GMLP_WIDTH = GMLP_GROUPS * GMLP_GROUP_DIM
CHUNK = 128
SPLIT_SIZES = (
    ATTN_WIDTH,
    KV_LATENT,
    IDX_HEADS * IDX_DIM,
    IDX_DIM,
    IDX_HEADS,
    2 * GMLP_WIDTH,
    2 * D_MODEL,
)
IN_COLS = int(sum(SPLIT_SIZES))
SPLIT_POINTS = tuple(int(v) for v in np.cumsum(SPLIT_SIZES)[:-1])
N_GROUPS = 8
EXPERTS_PER_GROUP = 8
N_EXPERTS = N_GROUPS * EXPERTS_PER_GROUP
TOPK_IN_GROUP = 2
EXPERT_FF = D_MODEL // 4
EPS = 1e-6

kernel_name = "hybrid_dsa_gmlp_hiermoe_block"


def rms_norm(x, g):
    xf = x.astype(jnp.float32)
    y = xf * lax.rsqrt(jnp.mean(xf * xf, axis=-1, keepdims=True) + EPS)
    return (y * g.astype(jnp.float32)).astype(x.dtype)


def layer_norm(x, g, b):
    xf = x.astype(jnp.float32)
    mu = jnp.mean(xf, axis=-1, keepdims=True)
    var = jnp.mean(jnp.square(xf - mu), axis=-1, keepdims=True)
    y = (xf - mu) * lax.rsqrt(var + EPS)
    return (y * g.astype(jnp.float32) + b.astype(jnp.float32)).astype(x.dtype)


def sparse_mla_attention(q, c_kv, q_idx, k_idx, w_idx, w_uk, w_uv):
    B, S = q.shape[0], q.shape[1]
    topk = min(TOPK_MAX, S // 4)
    nb = S // Q_BLOCK
    scale = HEAD_DIM ** -0.5
    q_abs = jnp.einsum('bthd,hdc->bthc', q, w_uk)

    def to_blocks(a):
        return a.reshape((B, nb, Q_BLOCK) + a.shape[2:]).swapaxes(0, 1)

    s_pos = jnp.arange(S)

    def block(args):
        i, qa, qi, wi = args
        t_pos = i * Q_BLOCK + jnp.arange(Q_BLOCK)
        dots = jnp.einsum('bthd,bsd->bths', qi, k_idx).astype(jnp.float32)
        score = jnp.einsum('bth,bths->bts', wi.astype(jnp.float32), jax.nn.relu(dots))
        causal = s_pos[None, :] <= t_pos[:, None]
        score = jnp.where(causal[None], score, -jnp.inf)
        _, idx = lax.top_k(score, topk)
        valid = idx <= t_pos[None, :, None]
        c_sel = jax.vmap(lambda c, ix: c[ix])(c_kv, idx)
        logits = jnp.einsum('bthc,btkc->bthk', qa, c_sel).astype(jnp.float32) * scale
        logits = jnp.where(valid[:, :, None, :], logits, -jnp.inf)
        p = jax.nn.softmax(logits, axis=-1).astype(c_sel.dtype)
        return jnp.einsum('bthk,btkc->bthc', p, c_sel)

    o = lax.map(block, (jnp.arange(nb), to_blocks(q_abs), to_blocks(q_idx), to_blocks(w_idx)))
    o = o.swapaxes(0, 1).reshape(B, S, N_HEADS, KV_LATENT)
    o = jnp.einsum('bthc,hcd->bthd', o, w_uv)
    return o.reshape(B, S, ATTN_WIDTH)


def chunked_spatial_gating(uv, w_s, b_s, ln_g, ln_b):
    B, S = uv.shape[0], uv.shape[1]
    z = jax.nn.gelu(uv)
    u, v = jnp.split(z, 2, axis=-1)
    v = layer_norm(v, ln_g, ln_b)
    v = v.reshape(B, S // CHUNK, CHUNK, GMLP_GROUPS, GMLP_GROUP_DIM)
    causal = jnp.tril(jnp.ones((CHUNK, CHUNK), dtype=w_s.dtype))
    y = jnp.einsum('gts,bnsgc->bntgc', w_s * causal[None], v) + b_s.T[None, None, :, :, None]
    return u * y.reshape(B, S, GMLP_WIDTH)


def hybrid_mixer(h, w_in, kv_norm_g, w_uk, w_uv, gmlp_ws, gmlp_bs, ln_v_g, ln_v_b,
                 w_br_attn, w_br_gmlp, w_out):
    B, S, _ = h.shape
    proj = jnp.einsum('bsd,de->bse', h, w_in)
    q, c_kv, q_idx, k_idx, w_idx, uv, gates = jnp.split(proj, SPLIT_POINTS, axis=-1)
    q = q.reshape(B, S, N_HEADS, HEAD_DIM)
    c_kv = rms_norm(c_kv, kv_norm_g)
    q_idx = q_idx.reshape(B, S, IDX_HEADS, IDX_DIM) * (IDX_DIM ** -0.5)
    w_idx = w_idx * (IDX_HEADS ** -0.5)
    a = sparse_mla_attention(q, c_kv, q_idx, k_idx, w_idx, w_uk, w_uv)
    m = chunked_spatial_gating(uv, gmlp_ws, gmlp_bs, ln_v_g, ln_v_b)
    g_a, g_b = jnp.split(gates, 2, axis=-1)
    merged = (jax.nn.sigmoid(g_a) * jnp.einsum('bsa,ad->bsd', a, w_br_attn)
              + jax.nn.sigmoid(g_b) * jnp.einsum('bsm,md->bsd', m, w_br_gmlp))
    return jnp.einsum('bsd,de->bse', merged, w_out)


def hierarchical_moe(h, w_group, b_group, w_router, b_router, w_e_gate, w_e_up, w_e_down):
    B, S, D = h.shape
    xt = h.reshape(B * S, D)
    n_tok = xt.shape[0]
    g_logits = jnp.einsum('nd,dg->ng', xt, w_group).astype(jnp.float32) + b_group.astype(jnp.float32)
    g_prob = jax.nn.softmax(g_logits, axis=-1)
    g_val, g_idx = lax.top_k(g_prob, 1)
    g_onehot = jax.nn.one_hot(g_idx[:, 0], N_GROUPS, dtype=jnp.float32)
    e_logits = (jnp.einsum('nd,de->ne', xt, w_router).astype(jnp.float32)
                + b_router.astype(jnp.float32)).reshape(n_tok, N_GROUPS, EXPERTS_PER_GROUP)
    sel = jnp.einsum('ng,nge->ne', g_onehot, e_logits)
    top_v, top_i = lax.top_k(sel, TOPK_IN_GROUP)
    q = jax.nn.softmax(top_v, axis=-1)
    within = jnp.sum(jax.nn.one_hot(top_i, EXPERTS_PER_GROUP, dtype=jnp.float32) * q[..., None], axis=1)
    combine = (g_onehot[:, :, None] * g_val[:, :, None] * within[:, None, :])
    combine = combine.reshape(n_tok, N_EXPERTS).astype(h.dtype)
    out = jnp.zeros_like(xt)
    for g in range(N_GROUPS):
        sl = slice(g * EXPERTS_PER_GROUP, (g + 1) * EXPERTS_PER_GROUP)
        hid = (jax.nn.silu(jnp.einsum('nd,edf->nef', xt, w_e_gate[sl]))
               * jnp.einsum('nd,edf->nef', xt, w_e_up[sl]) * combine[:, sl, None])
        out = out + jnp.einsum('nef,efd->nd', hid, w_e_down[sl])
    return out.reshape(B, S, D)


def setup_inputs(seed: int = 0) -> dict:
    key = jax.random.key(seed)
    ks = jax.random.split(key, 24)
    f32 = jnp.float32

    def nrm(k, shape, scale):
        return jax.random.normal(k, shape, f32) * scale

    return {
        "x": nrm(ks[0], (BATCH, SEQ, D_MODEL), 1.0),
        "norm1_g": 1.0 + nrm(ks[1], (D_MODEL,), 0.02),
        "w_in": nrm(ks[2], (D_MODEL, IN_COLS), D_MODEL ** -0.5),
        "kv_norm_g": 1.0 + nrm(ks[3], (KV_LATENT,), 0.02),
        "w_uk": nrm(ks[4], (N_HEADS, HEAD_DIM, KV_LATENT), KV_LATENT ** -0.5),
        "w_uv": nrm(ks[5], (N_HEADS, KV_LATENT, HEAD_DIM), KV_LATENT ** -0.5),
        "gmlp_ws": nrm(ks[6], (GMLP_GROUPS, CHUNK, CHUNK), CHUNK ** -0.5),
        "gmlp_bs": 1.0 + nrm(ks[7], (GMLP_GROUPS, CHUNK), 0.1),
        "ln_v_g": 1.0 + nrm(ks[8], (GMLP_WIDTH,), 0.02),
        "ln_v_b": nrm(ks[9], (GMLP_WIDTH,), 0.02),
        "w_br_attn": nrm(ks[10], (ATTN_WIDTH, D_MODEL), ATTN_WIDTH ** -0.5),
        "w_br_gmlp": nrm(ks[11], (GMLP_WIDTH, D_MODEL), GMLP_WIDTH ** -0.5),
        "w_out": nrm(ks[12], (D_MODEL, D_MODEL), D_MODEL ** -0.5),
        "norm2_g": 1.0 + nrm(ks[13], (D_MODEL,), 0.02),
        "w_group": nrm(ks[14], (D_MODEL, N_GROUPS), D_MODEL ** -0.5),
        "b_group": nrm(ks[15], (N_GROUPS,), 0.01),
        "w_router": nrm(ks[16], (D_MODEL, N_EXPERTS), D_MODEL ** -0.5),
        "b_router": nrm(ks[17], (N_EXPERTS,), 0.01),
        "w_e_gate": nrm(ks[18], (N_EXPERTS, D_MODEL, EXPERT_FF), D_MODEL ** -0.5),
        "w_e_up": nrm(ks[19], (N_EXPERTS, D_MODEL, EXPERT_FF), D_MODEL ** -0.5),
        "w_e_down": nrm(ks[20], (N_EXPERTS, EXPERT_FF, D_MODEL), EXPERT_FF ** -0.5),
        "norm_f_g": 1.0 + nrm(ks[21], (D_MODEL,), 0.02),
    }


def reference(x, norm1_g, w_in, kv_norm_g, w_uk, w_uv, gmlp_ws, gmlp_bs, ln_v_g, ln_v_b,
              w_br_attn, w_br_gmlp, w_out, norm2_g, w_group, b_group, w_router, b_router,
              w_e_gate, w_e_up, w_e_down, norm_f_g):
    h = x
    for _ in range(DEPTH):
        h = h + hybrid_mixer(rms_norm(h, norm1_g), w_in, kv_norm_g, w_uk, w_uv, gmlp_ws, gmlp_bs,
                             ln_v_g, ln_v_b, w_br_attn, w_br_gmlp, w_out)
        h = h + hierarchical_moe(rms_norm(h, norm2_g), w_group, b_group, w_router, b_router,
                                 w_e_gate, w_e_up, w_e_down)
    return rms_norm(h, norm_f_g)
```

```python
import numpy as np
import concourse.bass as bass
import concourse.mybir as mybir
from concourse.bass_utils import run_bass_kernel_spmd

F32 = mybir.dt.float32
BF16 = mybir.dt.bfloat16
AF = mybir.ActivationFunctionType
ALU = mybir.AluOpType
AX = mybir.AxisListType

NCORES = 8
S = 8192
D = 2048
NB = 64
LB = 8
KC = 16
IN_COLS = 8528
C_Q, C_KV, C_QI, C_KI, C_WI, C_UV, C_G = 0, 1024, 1280, 2304, 2368, 2384, 4432
EPS = 1e-6
TOPK = 256
NE = 64
FF = 512
BIS_R = 32.0
BIS_N = 24
NEG = -1.0e30


class Ev:
    __slots__ = ("kind", "eng", "rec", "sem", "val")

    def __init__(self, kind, eng=None, rec=None, sem=None, val=None):
        self.kind, self.eng, self.rec, self.sem, self.val = kind, eng, rec, sem, val


class Buf:
    def __init__(self, name):
        self.name = name
        self.w = None
        self.r = {}
        self.sem = None
        self.semval = 0


def _evkey(ev):
    return ("e", ev.eng) if ev.kind == "eng" else ("d", id(ev.sem))


def _evrank(ev):
    return ev.rec["idx"] if ev.kind == "eng" else ev.val


class V:
    def __init__(self, b, ap):
        self.b, self.ap = b, ap

    def __getitem__(self, idx):
        return V(self.b, self.ap[idx])

    def re(self, pat, **kw):
        return V(self.b, self.ap.rearrange(pat, **kw))

    def bc(self, shape):
        return V(self.b, self.ap.broadcast_to(shape))

    def cast(self, dt):
        return V(self.b, self.ap.bitcast(dt))


ENGS = ("pe", "act", "dve", "pool", "sp")


class Prog:
    def __init__(self, nc, arena, psum, sems):
        self.nc = nc
        self.arena = arena
        self.arena_words = arena.shape[1]
        self.top = 0
        self.psum = psum
        self.banks = [Buf("bank%d" % i) for i in range(8)]
        for b_ in self.banks:
            b_.excl = True
        self.streams = {e: [] for e in ENGS}
        self.free_sems = list(sems)
        self.eng_sem = {e: self.free_sems.pop() for e in ENGS}
        self.dma_events = {}

    def alloc(self, name, free_shape, dt):
        nelem = int(np.prod(free_shape))
        bpe = 4 if dt == F32 else 2
        words = (nelem * bpe + 3) // 4
        words = (words + 15) // 16 * 16
        assert self.top + words <= self.arena_words, ("SBUF arena overflow", name, self.top, words)
        ap = self.arena[:, self.top:self.top + words]
        self.top += words
        if dt != F32:
            ap = ap.bitcast(dt)
        ap = ap[:, 0:nelem]
        if len(free_shape) == 2:
            ap = ap.rearrange("p (a b) -> p a b", a=free_shape[0])
        elif len(free_shape) == 3:
            ap = ap.rearrange("p (a b c) -> p a b c", a=free_shape[0], b=free_shape[1])
        elif len(free_shape) == 4:
            ap = ap.rearrange("p (a b c d) -> p a b c d", a=free_shape[0], b=free_shape[1], c=free_shape[2])
        return V(Buf(name), ap)

    def bank(self, i, dt=F32, cols=512):
        assert dt == F32 and i < 8
        ap = self.psum[:, i * 512:(i + 1) * 512]
        return V(self.banks[i], ap[:, 0:cols])

    def tbank(self, i, cols=512):
        ap = self.psum_b[:, i * 1024:(i + 1) * 1024]
        return V(self.banks[6 + i], ap[:, 0:cols])

    def _rec(self, eng, fn, reads, writes, dma=False):
        rec = {"fn": fn, "waits": [], "sig": False, "cnt": None, "dma": None, "idx": len(self.streams[eng])}
        is_pe = eng == "pe"
        best = {}

        def need(ev, raw):
            if ev.kind == "eng" and ev.eng == eng and not dma:
                if is_pe or not raw:
                    return
            k = _evkey(ev)
            if k not in best or _evrank(best[k]) < _evrank(ev):
                best[k] = ev

        for v in reads:
            b = v.b if isinstance(v, V) else v
            if b.w is not None:
                need(b.w, True)
            if getattr(b, "excl", False):
                for e_ in b.r.values():
                    need(e_, False)
        for v in writes:
            b = v.b if isinstance(v, V) else v
            if b.w is not None:
                need(b.w, False)
            for e_ in b.r.values():
                need(e_, False)
        for ev in best.values():
            rec["waits"].append(ev)
            if ev.kind == "eng":
                ev.rec["sig"] = True
        if dma:
            owner = None
            for v in list(writes) + list(reads):
                b = v.b if isinstance(v, V) else v
                if not getattr(b, "is_dram", False):
                    owner = b
                    break
            assert owner is not None
            if owner.sem is None:
                owner.sem = self.free_sems.pop()
            owner.semval += 16
            ev = Ev("dma", sem=owner.sem, val=owner.semval)
            rec["dma"] = ev
            k = _evkey(ev)
            self.dma_events[k] = ev
        else:
            ev = Ev("eng", eng=eng, rec=rec)
        for v in reads:
            b = v.b if isinstance(v, V) else v
            b.r[_evkey(ev)] = ev
        for v in writes:
            b = v.b if isinstance(v, V) else v
            b.w = ev
            b.r = {}
        self.streams[eng].append(rec)
        return ev

    def barrier(self):
        lasts = {}
        for e in ENGS:
            for rec in reversed(self.streams[e]):
                if rec["dma"] is None and rec["fn"] is not None:
                    lasts[e] = Ev("eng", eng=e, rec=rec)
                    break
        dmas = list(self.dma_events.values())
        self.dma_events = {}
        for e in ENGS:
            rec = {"fn": None, "waits": [], "sig": False, "cnt": None, "dma": None, "idx": len(self.streams[e])}
            for f, ev in lasts.items():
                if f != e:
                    rec["waits"].append(ev)
                    ev.rec["sig"] = True
            rec["waits"].extend(dmas)
            self.streams[e].append(rec)

    def emit(self, eng, e):
        sem_e = self.eng_sem[eng]
        waited = {}
        for rec in self.streams[eng]:
            for ev in rec["waits"]:
                if ev.kind == "eng":
                    sem, val = self.eng_sem[ev.eng], ev.rec["cnt"]
                else:
                    sem, val = ev.sem, ev.val
                key = id(sem)
                if waited.get(key, 0) >= val:
                    continue
                waited[key] = val
                e.wait_ge(sem, val)
            if rec["fn"] is None:
                continue
            ins = rec["fn"](e)
            if rec["dma"] is not None:
                ins.then_inc(rec["dma"].sem, 16)
            elif rec["sig"]:
                ins.then_inc(sem_e, 1)

    def finalize(self):
        for eng in ENGS:
            n = 0
            for rec in self.streams[eng]:
                if rec["sig"]:
                    n += 1
                    rec["cnt"] = n

    @staticmethod
    def _a(x):
        return x.ap if isinstance(x, V) else x

    def mm(self, out, lhsT, rhs, start=True, stop=True):
        o, l, r = out.ap, lhsT.ap, rhs.ap
        self._rec("pe", lambda e: e.matmul(o, l, r, start=start, stop=stop), [lhsT, rhs], [out])

    def tr(self, out, in_, ident):
        o, i, d = out.ap, in_.ap, ident.ap
        self._rec("pe", lambda e: e.matmul(o, i, d, start=True, stop=True), [in_, ident], [out])

    def act(self, out, in_, func, scale=1.0, bias=None, accum=None):
        o, i = out.ap, in_.ap
        reads = [in_]
        writes = [out]
        kw = {}
        if isinstance(scale, V):
            reads.append(scale)
        kw["scale"] = self._a(scale)
        if bias is not None:
            if isinstance(bias, V):
                reads.append(bias)
            kw["bias"] = self._a(bias)
        if accum is not None:
            writes.append(accum)
            kw["accum_out"] = accum.ap
        self._rec("act", lambda e: e.activation(o, i, func, **kw), reads, writes)

    def ts(self, eng, out, in0, s1, op0, s2=None, op1=None, accum=None):
        o, i = out.ap, in0.ap
        reads = [in0] + [s for s in (s1, s2) if isinstance(s, V)]
        writes = [out]
        a1, a2 = self._a(s1), self._a(s2)
        kw = {}
        if op1 is not None:
            kw["op1"] = op1
        if accum is not None:
            writes.append(accum)
            kw["accum_out"] = accum.ap
        self._rec(eng, lambda e: e.tensor_scalar(o, i, a1, a2, op0, **kw), reads, writes)

    def tt(self, eng, out, in0, in1, op):
        o, a, b = out.ap, in0.ap, in1.ap
        self._rec(eng, lambda e: e.tensor_tensor(o, a, b, op), [in0, in1], [out])

    def stt(self, out, in0, scalar, in1, op0, op1):
        o, a, b = out.ap, in0.ap, in1.ap
        reads = [in0, in1] + ([scalar] if isinstance(scalar, V) else [])
        s = self._a(scalar)
        self._rec("dve", lambda e: e.scalar_tensor_tensor(o, a, s, b, op0, op1), reads, [out])

    def copy(self, eng, out, in_):
        o, i = out.ap, in_.ap
        if eng == "act":
            self._rec("act", lambda e: e.activation(o, i, AF.Identity), [in_], [out])
        else:
            self._rec(eng, lambda e: e.tensor_copy(o, i), [in_], [out])

    def recip(self, out, in_):
        o, i = out.ap, in_.ap
        self._rec("dve", lambda e: e.reciprocal(o, i), [in_], [out])

    def reduce(self, out, in_, op, axis=AX.X):
        o, i = out.ap, in_.ap
        self._rec("dve", lambda e: e.tensor_reduce(o, i, axis, op), [in_], [out])

    def memset(self, eng, out, val):
        o = out.ap
        self._rec(eng, lambda e: e.memset(o, val), [], [out])

    def iota(self, out, pattern, base, cm):
        o = out.ap
        self._rec("pool", lambda e: e.iota(o, pattern, base=base, channel_multiplier=cm,
                                           allow_small_or_imprecise_dtypes=True), [], [out])

    def dma(self, out, in_, q="sp"):
        o, i = out.ap, in_.ap
        return self._rec(q, lambda e: e.dma_start(out=o, in_=i), [in_], [out], dma=True)


class DBuf(Buf):
    is_dram = True


def dram_v(nc, name, shape, dt, kind):
    t = nc.dram_tensor(name, list(shape), dt, kind=kind)
    return V(DBuf(name), t.ap())


def build(debug=False, n_experts=NE, stop=None, nb_p0=None):
    nc = bass.Bass("TRN2", target_bir_lowering=False)
    I = lambda n, s, dt=F32: dram_v(nc, n, s, dt, "ExternalInput")
    xT_all = I("xT_all", [NB, 128, 2048])
    xT_loc = I("xT_loc", [LB, 128, 2048])
    x_loc = I("x_loc", [LB, 128, 2048])
    trel_d = I("trel", [128, 1])
    g1_d = I("g1", [128, KC])
    w_in = I("w_in", [128, KC, IN_COLS])
    gkv_d = I("gkv", [256])
    w_uk = I("w_uk", [8, 128, 256])
    w_uv = I("w_uv", [8, 256, 128])
    wsT_d = I("wsT", [8, 128, 128])
    bs_d = I("bs", [1024])
    lng_d = I("lng", [1024])
    lnb_d = I("lnb", [1024])
    w_bra = I("w_bra", [128, 8, 2048])
    w_brg = I("w_brg", [128, 8, 2048])
    w_out = I("w_out", [128, KC, 2048])
    g2_d = I("g2", [128, KC])
    wr_d = I("wr", [128, KC, 72])
    br_d = I("br", [72])
    weg = I("weg", [n_experts, 2, 128, KC, 256])
    weu = I("weu", [n_experts, 2, 128, KC, 256])
    wed = I("wed", [n_experts, 128, 4, 2048])
    gf_d = I("gf", [2048])
    out_d = dram_v(nc, "out", [LB, 128, 2048], F32, "ExternalOutput")
    ckv_s = dram_v(nc, "ckv_s", [NB, 128, 256], BF16, "Internal")
    qT_s = dram_v(nc, "qT_s", [LB, 128, 1024], BF16, "Internal")
    qiT_s = dram_v(nc, "qiT_s", [LB, 128, 1024], BF16, "Internal")
    dbg = {}
    if debug:
        dbg["ckvT"] = dram_v(nc, "dbg_ckvT", [128, 2, S], BF16, "ExternalOutput")
        dbg["kT"] = dram_v(nc, "dbg_kT", [128, S], BF16, "ExternalOutput")
        dbg["aT"] = dram_v(nc, "dbg_aT", [128, 8, 1024], BF16, "ExternalOutput")
        dbg["mT"] = dram_v(nc, "dbg_mT", [128, 8, 1024], BF16, "ExternalOutput")
        dbg["h"] = dram_v(nc, "dbg_h", [128, LB, 2048], F32, "ExternalOutput")
        dbg["thr"] = dram_v(nc, "dbg_thr", [128, LB], F32, "ExternalOutput")
        dbg["comb"] = dram_v(nc, "dbg_comb", [128, LB, 64], F32, "ExternalOutput")

    import contextlib
    with contextlib.ExitStack() as st:
        arena = st.enter_context(nc.sbuf_tensor("arena", [128, 207 * 256], F32))
        psum = st.enter_context(nc.psum_tensor("psum", [128, 4096], F32))
        sems = [st.enter_context(nc.semaphore("s%d" % i)) for i in range(96)]
        P = Prog(nc, arena, psum, sems)
        program(P, locals())
        P.finalize()
        block = st.enter_context(nc.Block())

        @block.tensor
        def _(e):
            P.emit("pe", e)

        @block.scalar
        def _(e):
            P.emit("act", e)

        @block.vector
        def _(e):
            P.emit("dve", e)

        @block.gpsimd
        def _(e):
            P.emit("pool", e)

        @block.sync
        def _(e):
            P.emit("sp", e)
    return nc


def program(P, T):
    xT_all, xT_loc, x_loc, w_in = T["xT_all"], T["xT_loc"], T["x_loc"], T["w_in"]
    out_d, dbg = T["out_d"], T["dbg"]
    blocks = lambda v, n: [V(DBuf("%s%d" % (v.b.name, i)), v.ap[i]) for i in range(n)]
    ckv_s = blocks(T["ckv_s"], NB)
    qT_s = blocks(T["qT_s"], LB)
    qiT_s = blocks(T["qiT_s"], LB)
    KB = lambda kb: kb * 256
    rr = [0]

    def eng2():
        rr[0] ^= 1
        return "act" if rr[0] else "dve"

    def at(kb):
        P.top = KB(kb)

    def rot(lst):
        c = [0]

        def f():
            c[0] += 1
            return lst[(c[0] - 1) % len(lst)]
        return f

    def bcast(v):
        return V(v.b, v.ap.partition_broadcast(128))

    ident_f = P.alloc("ident_f", (128,), F32)
    ident_b = P.alloc("ident_b", (128,), BF16)
    ones128 = P.alloc("ones128", (128,), BF16)
    iot = P.alloc("iot", (128,), F32)
    tri = P.alloc("tri", (128,), F32)
    g1 = P.alloc("g1", (KC,), F32)
    g2 = P.alloc("g2", (KC,), F32)
    trel = P.alloc("trel", (1,), F32)
    epsb = P.alloc("epsb", (1,), F32)
    gkv_bc = P.alloc("gkv_bc", (256,), F32)
    wi = P.alloc("wi", (LB, 16), F32)
    rs_tm = P.alloc("rs_tm", (LB,), F32)
    rs_bc = P.alloc("rs_bc", (1024,), F32)
    comb = P.alloc("comb", (LB, 64), F32)
    thr = P.alloc("thr", (LB,), F32)
    off_wuk = P.top
    wuk = P.alloc("wuk", (8, 256), BF16)
    wuv = P.alloc("wuv", (8, 2, 128), BF16)
    assert P.top <= KB(20), P.top
    P.iota(iot, [[1, 128]], 0, -1)
    P.ts("pool", ident_f, iot, 0.0, ALU.is_equal)
    P.copy("pool", ident_b, ident_f)
    P.memset("pool", ones128, 1.0)
    P.memset("pool", epsb, EPS)
    P.ts("pool", tri, iot, 0.0, ALU.is_ge)
    P.dma(g1, T["g1_d"])
    P.dma(g2, T["g2_d"])
    P.dma(trel, T["trel_d"])
    P.dma(gkv_bc, bcast(T["gkv_d"]))
    if T.get('stop') == -1:
        P.barrier()
        return
    at(20)
    aT = P.alloc("aT", (8, 1024), BF16)
    ckvT = P.alloc("ckvT", (2, S), BF16)
    kT = P.alloc("kT", (S,), BF16)
    assert P.top == KB(84), P.top

    def load_w_cols(dst, c0, c1, stg2, fold=True):
        n = c1 - c0
        for k0 in range(0, KC, 4):
            stg = stg2[(k0 // 4) % 2].re("p k t -> p (k t)").re("p (a b) -> p a b", a=4)
            P.dma(stg[:, :, 0:n], w_in[:, k0:k0 + 4, c0:c1])
            for k in range(k0, k0 + 4):
                if eng2() == "act":
                    P.act(dst[:, k, 0:n], stg[:, k - k0, 0:n], AF.Identity, scale=g1[:, k:k + 1])
                else:
                    P.ts("dve", dst[:, k, 0:n], stg[:, k - k0, 0:n], g1[:, k:k + 1], ALU.mult)

    at(84)
    xst = [P.alloc("xst%d" % i, (KC, 128), F32) for i in range(2)]
    wkv = P.alloc("wkv", (KC, 320), BF16)
    load_w_cols(wkv[:, :, 0:256], C_KV, C_KV + 256, xst)
    load_w_cols(wkv[:, :, 256:320], C_KI, C_KI + 64, xst)
    if T.get('stop') == -2:
        P.barrier()
        return
    xb = [P.alloc("xb%d" % i, (KC, 128), BF16) for i in range(2)]
    xsq = [P.alloc("xsq%d" % i, (KC, 128), BF16) for i in range(2)]
    sm = [dict((n, P.alloc("%s%d" % (n, i), (1,), F32)) for n in ("sd", "rs", "sv", "mv", "rv", "rr"))
          for i in range(2)]
    junk = [P.alloc("junk%d" % i, (256,), F32) for i in range(2)]
    ckv_tm = [P.alloc("ckv_tm%d" % i, (256,), BF16) for i in range(2)]
    k_tm = [P.alloc("k_tm%d" % i, (128,), BF16) for i in range(2)]
    for i in range(2):
        P.memset("pool", k_tm[i], 0.0)
    def p0_front(b):
        i = b % 2
        P.dma(xst[i].re("p k t -> p (k t)"), xT_all[b])
        P.copy("dve", xb[i], xst[i])
        P.act(xsq[i], xst[i], AF.Square)
        kv = P.bank(i)
        ss = P.bank(2 + i)
        for k in range(KC):
            P.mm(kv[:, 0:320], xb[i][:, k, :], wkv[:, k, :], start=(k == 0), stop=(k == KC - 1))
        for k in range(KC):
            P.mm(ss[:, 0:1], xsq[i][:, k, :], ones128[:, 0:1], start=(k == 0), stop=(k == KC - 1))

    def p0_back(b):
        i = b % 2
        kv = P.bank(i)
        ss = P.bank(2 + i)
        s = sm[i]
        P.act(s["sd"], ss[:, 0:1], AF.Sqrt, scale=1.0 / D, bias=epsb)
        P.recip(s["rs"], s["sd"])
        P.act(junk[i], kv[:, 0:256], AF.Square, accum=s["sv"])
        P.ts("dve", s["mv"], s["sv"], s["rs"], ALU.mult, s["rs"], ALU.mult)
        P.act(s["rv"], s["mv"], AF.Sqrt, scale=1.0 / 256, bias=epsb)
        P.recip(s["rr"], s["rv"])
        P.tt("dve", s["rr"], s["rr"], s["rs"], ALU.mult)
        P.stt(ckv_tm[i], kv[:, 0:256], s["rr"], gkv_bc, ALU.mult, ALU.mult)
        P.act(k_tm[i][:, 0:64], kv[:, 256:320], AF.Identity, scale=s["rs"])
        P.dma(ckv_s[b], ckv_tm[i])
        tp = P.bank(4 + i, F32, 384)
        P.tr(tp[:, 0:128], ckv_tm[i][:, 0:128], ident_b)
        P.tr(tp[:, 128:256], ckv_tm[i][:, 128:256], ident_b)
        P.tr(tp[:, 256:384], k_tm[i], ident_b)
        P.copy("dve", ckvT[:, :, b * 128:(b + 1) * 128], tp[:, 0:256].re("p (c t) -> p c t", c=2))
        P.copy("act", kT[0:64, b * 128:(b + 1) * 128], tp[0:64, 256:384])

    nb0 = T.get('nb_p0') or NB
    p0_front(0)
    for b in range(nb0):
        if b + 1 < nb0:
            p0_front(b + 1)
        p0_back(b)
    if T.get('stop') in (-3, -4, -5):
        P.barrier()
        return
    P.dma(kT[64:128, :], kT[0:64, :])
    if dbg:
        P.dma(dbg["ckvT"], ckvT)
        P.dma(dbg["kT"], kT)
    P.barrier()
    if T.get('stop') == 0:
        return

    bank_rr = [0]

    def load_local_x(xTl, xst, stats):
        xsq1 = [P.alloc("xsq1_%d" % i, (KC, 128), BF16) for i in range(2)] if stats else None
        sd1 = [P.alloc("sd1_%d" % i, (1,), F32) for i in range(2)] if stats else None
        sdb = [P.alloc("sdb_%d" % i, (128,), F32) for i in range(2)] if stats else None
        for j in range(LB):
            i = j % 2
            P.dma(xst[i].re("p k t -> p (k t)"), xT_loc[j])
            P.copy("dve", xTl[:, :, j * 128:(j + 1) * 128], xst[i])
            if not stats:
                continue
            P.act(xsq1[i], xst[i], AF.Square)
            ss = P.bank(2 + i)
            for k in range(KC):
                P.mm(ss[:, 0:1], xsq1[i][:, k, :], ones128[:, 0:1], start=(k == 0), stop=(k == KC - 1))
            P.act(sd1[i], ss[:, 0:1], AF.Sqrt, scale=1.0 / D, bias=epsb)
            P.recip(rs_tm[:, j:j + 1], sd1[i])
            sb_ = P.bank(i)
            for k in range(KC):
                P.mm(sb_[:, 0:128], ones128, xsq1[i][:, k, :], start=(k == 0), stop=(k == KC - 1))
            P.act(sdb[i], sb_[:, 0:128], AF.Sqrt, scale=1.0 / D, bias=epsb)
            P.recip(rs_bc[:, j * 128:(j + 1) * 128], sdb[i])

    def fm_proj(wq, xTl, xst, c0, ncols, evac):
        load_w_cols(wq, c0, c0 + ncols, xst)
        for m in range(ncols // 128):
            for half in range(2):
                bk = P.bank(bank_rr[0] % 4)
                bank_rr[0] += 1
                for k in range(KC):
                    P.mm(bk, wq[:, k, m * 128:(m + 1) * 128], xTl[:, k, half * 512:(half + 1) * 512],
                         start=(k == 0), stop=(k == KC - 1))
                evac(m, half, bk)

    at(84)
    xTl = P.alloc("xTl", (KC, 1024), BF16)
    xst = [P.alloc("xst%d" % i, (KC, 128), F32) for i in range(2)]
    next_wq = rot([P.alloc("wq%d" % i, (KC, 512), BF16) for i in range(2)])
    qT_st = P.alloc("qT_st", (4, 1024), BF16)
    wwi = P.alloc("wwi", (KC, 16), BF16)
    load_local_x(xTl, xst, True)
    su = xst[0].re("p k t -> p (k t)").re("p (h c) -> p h c", h=8)
    P.dma(su, V(T["w_uk"].b, T["w_uk"].ap.rearrange("h d c -> d h c")))
    P.copy("pool", wuk, su)
    sv_ = xst[1].re("p k t -> p (k t)").re("p (h cc d) -> p h cc d", h=8, cc=2)
    for h in range(8):
        P.dma(sv_[:, h, :, :], V(T["w_uv"].b, T["w_uv"].ap[h].rearrange("(cc p) d -> p cc d", p=128)))
    P.copy("pool", wuv, sv_)
    for (c0, dst, sc) in ((C_Q, qT_s, 1.0), (C_QI, qiT_s, 0.125)):
        for grp in range(2):
            def evac(m, half, bk, sc=sc):
                P.stt(qT_st[:, m, half * 512:(half + 1) * 512], bk, sc, rs_bc[:, half * 512:(half + 1) * 512],
                      ALU.mult, ALU.mult)
            fm_proj(next_wq(), xTl, xst, c0 + grp * 512, 512, evac)
            for j in range(LB):
                P.dma(dst[j].re("p (m t) -> p m t", m=8)[:, grp * 4:(grp + 1) * 4, :],
                      qT_st[:, :, j * 128:(j + 1) * 128])
    load_w_cols(wwi, C_WI, C_WI + 16, xst)
    for j in range(LB):
        bk = P.bank(4 + j % 2)
        for k in range(KC):
            P.mm(bk[:, 0:16], xTl[:, k, j * 128:(j + 1) * 128], wwi[:, k, :], start=(k == 0), stop=(k == KC - 1))
        P.ts("dve", wi[:, j, :], bk[:, 0:16], rs_tm[:, j:j + 1], ALU.mult, 0.25, ALU.mult)
    P.barrier()
    if T.get('stop') == 1:
        return

    at(84)
    srel = P.alloc("srel", (1024,), F32)
    negm = P.alloc("negm", (1024,), F32)
    score = P.alloc("score", (S,), F32)
    maskT = P.alloc("maskT", (NB, 128), BF16)
    qT_b = P.alloc("qT_b", (8, 128), BF16)
    qiT_b = P.alloc("qiT_b", (8, 128), BF16)
    qa_b = P.alloc("qa_b", (2, 8, 128), BF16)
    Dh = P.alloc("Dh", (16, 128), BF16)
    Rb = [P.alloc("R%d" % i, (512,), BF16) for i in range(4)]
    PTb = [P.alloc("PT%d" % i, (4, 128), BF16) for i in range(3)]
    ckvb = [P.alloc("ckvb%d" % i, (256,), BF16) for i in range(4)]
    mk = [P.alloc("mk%d" % i, (512,), BF16) for i in range(2)]
    cnt = P.alloc("cnt", (1,), F32)
    mid = P.alloc("mid", (1,), F32)
    t1 = P.alloc("t1", (1,), F32)
    bjunk = P.alloc("bjunk", (S,), BF16)
    rZ = P.alloc("rZ", (512,), F32)
    OTn = P.alloc("OTn", (2, 4, 128), BF16)
    P.iota(srel, [[1, 1024]], 0, 0)
    P.ts("dve", negm, srel, trel[:, 0:1], ALU.is_gt, NEG, ALU.mult)
    SCALE = 128 ** -0.5
    cntr = {"ri": 0, "pti": 0, "cki": 0}
    rZ2 = [rZ, P.alloc("rZb", (512,), F32)]
    OTn2 = [OTn, P.alloc("OTnb", (2, 4, 128), BF16)]

    def p2_idx(j):
        nblk = 8 * (j + 1)
        P.dma(qiT_b.re("p m t -> p (m t)"), qiT_s[j])
        for h in range(16):
            P.ts("pool", Dh[:, h, :], ident_f, wi[:, j, h:h + 1], ALU.mult)
        for sg in range(nblk // 4):
            sc_ps = P.bank(2 + sg % 2)

            def emit_dots(h, sg=sg):
                dps = P.bank(h % 2)
                lo = 64 * (h % 2)
                P.mm(dps, qiT_b[lo:lo + 64, h // 2, :], kT[lo:lo + 64, sg * 512:(sg + 1) * 512])
                R = Rb[cntr["ri"] % 4]
                cntr["ri"] += 1
                if h % 2 == 0:
                    P.act(R, dps, AF.Relu)
                else:
                    P.ts("dve", R, dps, 0.0, ALU.max)
                return R

            Rp = emit_dots(0)
            for h in range(16):
                Rn = emit_dots(h + 1) if h + 1 < 16 else None
                P.mm(sc_ps, Dh[:, h, :], Rp, start=(h == 0), stop=(h == 15))
                Rp = Rn
            if sg >= 2 * j:
                o = (sg - 2 * j) * 512
                P.tt("dve", score[:, sg * 512:(sg + 1) * 512], sc_ps, negm[:, o:o + 512], ALU.add)
            else:
                P.copy("act", score[:, sg * 512:(sg + 1) * 512], sc_ps)

    def p2_bis(j):
        Sj = 8 * (j + 1) * 128
        P.memset("dve", mid, 0.0)
        for it in range(BIS_N):
            step = BIS_R / (2 ** (it + 1))
            P.ts("dve", bjunk[:, 0:Sj], score[:, 0:Sj], mid[:, 0:1], ALU.is_ge, 0.0, ALU.add, accum=cnt)
            P.ts("dve", t1, cnt, float(TOPK), ALU.is_ge, 2.0 * step, ALU.mult)
            P.ts("dve", mid, t1, -step, ALU.add, mid[:, 0:1], ALU.add)
        P.ts("dve", thr[:, j:j + 1], mid, -BIS_R / (2 ** BIS_N), ALU.add)

    def p2_mask(j):
        nblk = 8 * (j + 1)
        for sg in range(nblk // 4):
            m_ = mk[sg % 2]
            P.ts("dve", m_, score[:, sg * 512:(sg + 1) * 512], thr[:, j:j + 1], ALU.is_ge)
            tp = P.bank(5, F32, 512)
            for q in range(4):
                P.tr(tp[:, q * 128:(q + 1) * 128], m_[:, q * 128:(q + 1) * 128], ident_b)
            P.ts("dve", maskT[:, sg * 4:(sg + 1) * 4, :], tp.re("p (q t) -> p q t", q=4), 640.0, ALU.mult, -640.0, ALU.add)

    def p2_attn(j):
        nblk = 8 * (j + 1)
        P.dma(qT_b.re("p m t -> p (m t)"), qT_s[j])
        for cc in range(2):
            for hq in range(2):
                bk = P.bank(2 + cc * 2 + hq)
                for hh in range(4):
                    h = hq * 4 + hh
                    P.mm(bk[:, hh * 128:(hh + 1) * 128], wuk[:, h, cc * 128:(cc + 1) * 128], qT_b[:, h, :])
                P.copy("act", qa_b[:, cc, hq * 4:(hq + 1) * 4, :], bk.re("p (h t) -> p h t", h=4))
        for hq in range(2):
            OT = [P.bank(2 + 3 * hq), P.bank(3 + 3 * hq)]
            Z = P.bank(4 + 3 * hq)
            def emit_lg(i, hq=hq):
                cb = ckvb[cntr["cki"] % 4]
                cntr["cki"] += 1
                P.dma(cb, ckv_s[i])
                lg = P.bank(i % 2)
                for cc in range(2):
                    P.mm(lg, ckvT[:, cc, i * 128:(i + 1) * 128],
                         qa_b[:, cc, hq * 4:(hq + 1) * 4, :].re("p h t -> p (h t)"),
                         start=(cc == 0), stop=False)
                P.mm(lg.re("p (h t) -> p h t", h=4), ident_b,
                     maskT[:, i, :].re("p (o t) -> p o t", o=1).bc([128, 4, 128]), start=False, stop=True)
                PT = PTb[cntr["pti"] % 3]
                cntr["pti"] += 1
                P.act(PT.re("p h t -> p (h t)"), lg, AF.Exp, scale=SCALE)
                return cb, PT.re("p h t -> p (h t)")

            pend = emit_lg(0)
            for i in range(nblk):
                nxt = emit_lg(i + 1) if i + 1 < nblk else None
                cb, PTf = pend
                for cc in range(2):
                    P.mm(OT[cc], cb[:, cc * 128:(cc + 1) * 128], PTf, start=(i == 0), stop=(i == nblk - 1))
                P.mm(Z, ones128, PTf, start=(i == 0), stop=(i == nblk - 1))
                pend = nxt
        for hq in range(2):
            OT = [P.bank(2 + 3 * hq), P.bank(3 + 3 * hq)]
            Z = P.bank(4 + 3 * hq)
            P.recip(rZ2[hq], Z)
            for cc in range(2):
                P.tt("dve", OTn2[hq][:, cc, :, :].re("p h t -> p (h t)"), OT[cc], rZ2[hq], ALU.mult)
            ab = P.bank(hq)
            for hh in range(4):
                h = hq * 4 + hh
                for cc in range(2):
                    P.mm(ab[:, hh * 128:(hh + 1) * 128], wuv[:, h, cc, :], OTn2[hq][:, cc, hh, :],
                         start=(cc == 0), stop=(cc == 1))
            P.copy("act", aT[:, hq * 4:(hq + 1) * 4, j * 128:(j + 1) * 128], ab.re("p (h t) -> p h t", h=4))

    p2_idx(0)
    p2_bis(0)
    p2_mask(0)
    for j in range(LB):
        if j + 1 < LB:
            p2_idx(j + 1)
            p2_bis(j + 1)
        p2_attn(j)
        if j + 1 < LB:
            p2_mask(j + 1)
    if dbg:
        P.dma(dbg["aT"], aT)
        P.dma(dbg["thr"], thr)
    P.barrier()
    if T.get('stop') == 2:
        return

    at(36)
    mT = P.alloc("mT", (8, 1024), BF16)
    xTl = P.alloc("xTl", (KC, 1024), BF16)
    xst = [P.alloc("xst%d" % i, (KC, 128), F32) for i in range(2)]
    next_wq = rot([P.alloc("wq%d" % i, (KC, 512), BF16) for i in range(2)])
    gtmp = [dict((n, P.alloc("g%s%d" % (n, i), (512,), F32)) for n in ("x", "a", "b")) for i in range(2)]
    wsT = P.alloc("wsT", (8, 128), BF16)
    stgw = P.alloc("stgw", (8, 128), F32)
    bs_bc = P.alloc("bs_bc", (8, 128), F32)
    lng = P.alloc("lng", (1024,), F32)
    lnb = P.alloc("lnb", (1024,), F32)
    vtm = P.alloc("vtm", (LB, 1024), F32)
    vn = [P.alloc("vn%d" % i, (1024,), BF16) for i in range(2)]
    vj = P.alloc("vjunk", (1024,), F32)
    lst = [dict((n, P.alloc("l%s%d" % (n, i), (1,), F32)) for n in ("s1", "s2", "mu", "var", "sd", "rs"))
           for i in range(2)]
    ug = P.alloc("ug", (512,), F32)
    load_local_x(xTl, xst, False)
    gi = [0]

    def gelu_to(dst, src_ps, rs_operand, token_major, final_mul=None):
        g = gtmp[gi[0] % 2]
        gi[0] += 1
        if token_major:
            P.act(g["x"], src_ps, AF.Identity, scale=rs_operand)
        else:
            P.tt("dve", g["x"], src_ps, rs_operand, ALU.mult)
        P.act(g["a"], g["x"], AF.Square)
        P.ts("dve", g["a"], g["a"], 0.044715 * 1.5957691216, ALU.mult, 1.5957691216, ALU.add)
        P.tt("pool", g["b"], g["a"], g["x"], ALU.mult)
        P.act(g["a"], g["b"], AF.Sigmoid)
        if final_mul is None:
            P.tt("dve", dst, g["a"], g["x"], ALU.mult)
        else:
            P.tt("dve", g["b"], g["a"], g["x"], ALU.mult)
            P.tt("dve", dst, g["b"], final_mul, ALU.mult)

    P.dma(stgw, V(T["wsT_d"].b, T["wsT_d"].ap.rearrange("g s t -> s g t")))
    P.tt("pool", wsT, stgw, tri.re("p (o t) -> p o t", o=1).bc([128, 8, 128]), ALU.mult)
    P.dma(bs_bc.re("p g t -> p (g t)"), bcast(T["bs_d"]))
    P.dma(lng, bcast(T["lng_d"]))
    P.dma(lnb, bcast(T["lnb_d"]))
    for grp in range(2):
        wq = next_wq()
        load_w_cols(wq, C_UV + 1024 + grp * 512, C_UV + 1024 + (grp + 1) * 512, xst)
        for j in range(LB):
            bk = P.bank(bank_rr[0] % 4)
            bank_rr[0] += 1
            for k in range(KC):
                P.mm(bk, xTl[:, k, j * 128:(j + 1) * 128], wq[:, k, :], start=(k == 0), stop=(k == KC - 1))
            gelu_to(vtm[:, j, grp * 512:(grp + 1) * 512], bk, rs_tm[:, j:j + 1], True)
    for j in range(LB):
        i = j % 2
        l = lst[i]
        P.act(vj, vtm[:, j, :], AF.Identity, accum=l["s1"])
        P.act(vj, vtm[:, j, :], AF.Square, accum=l["s2"])
        P.ts("dve", l["mu"], l["s1"], 1.0 / 1024, ALU.mult)
        P.ts("dve", l["var"], l["mu"], l["mu"], ALU.mult, -1.0, ALU.mult)
        P.stt(l["var"], l["s2"], 1.0 / 1024, l["var"], ALU.mult, ALU.add)
        P.act(l["sd"], l["var"], AF.Sqrt, bias=epsb)
        P.recip(l["rs"], l["sd"])
        P.ts("dve", vj, vtm[:, j, :], l["mu"], ALU.subtract, l["rs"], ALU.mult)
        P.tt("dve", vj, vj, lng, ALU.mult)
        P.tt("dve", vn[i], vj, lnb, ALU.add)
        for gq in range(2):
            bk = P.bank(4 + gq)
            for gg in range(4):
                g = gq * 4 + gg
                P.mm(bk[:, gg * 128:(gg + 1) * 128], vn[i][:, g * 128:(g + 1) * 128], wsT[:, g, :])
            P.tt("dve", mT[:, gq * 4:(gq + 1) * 4, j * 128:(j + 1) * 128], bk.re("p (g t) -> p g t", g=4),
                 bs_bc[:, gq * 4:(gq + 1) * 4, :], ALU.add)
    for grp in range(2):
        def evac(m, half, bk, grp=grp):
            dst = mT[:, grp * 4 + m, half * 512:(half + 1) * 512]
            gelu_to(dst, bk, rs_bc[:, half * 512:(half + 1) * 512], False, final_mul=dst)
        fm_proj(next_wq(), xTl, xst, C_UV + grp * 512, 512, evac)
    if dbg:
        P.dma(dbg["mT"], mT)
    P.barrier()
    if T.get('stop') == 3:
        return

    at(84)
    xst = [P.alloc("xst%d" % i, (KC, 128), F32) for i in range(2)]
    mergedT = P.alloc("mergedT", (KC, 1024), BF16)
    next_wq = rot([P.alloc("wq%d" % i, (KC, 512), BF16) for i in range(2)])
    next_wbr = rot([P.alloc("wbr%d" % i, (8, 512), BF16) for i in range(2)])
    sig = [P.alloc("sig%d" % i, (512,), F32) for i in range(2)]
    prodA = [P.alloc("prodA%d" % i, (512,), F32) for i in range(2)]
    si = [0]

    def load_br(dst, src, c0):
        for k0 in range(0, 8, 4):
            s_ = xst[(k0 // 4) % 2].re("p k t -> p (k t)").re("p (a b) -> p a b", a=4)
            P.dma(s_, src[:, k0:k0 + 4, c0:c0 + 512])
            P.copy(eng2(), dst[:, k0:k0 + 4, :], s_)

    for dg in range(4):
        for br in range(2):
            wq = next_wq()
            wbr = next_wbr()
            load_br(wbr, T["w_bra"] if br == 0 else T["w_brg"], dg * 512)
            load_w_cols(wq, C_G + br * 2048 + dg * 512, C_G + br * 2048 + (dg + 1) * 512, xst)
            src = aT if br == 0 else mT
            for m in range(4):
                for half in range(2):
                    hs = slice(half * 512, (half + 1) * 512)
                    gb = P.bank(bank_rr[0] % 4)
                    bb = P.bank(4 + bank_rr[0] % 2)
                    bank_rr[0] += 1
                    for k in range(KC):
                        P.mm(gb, wq[:, k, m * 128:(m + 1) * 128], xTl[:, k, hs], start=(k == 0), stop=(k == KC - 1))
                    for k in range(8):
                        P.mm(bb, wbr[:, k, m * 128:(m + 1) * 128], src[:, k, hs], start=(k == 0), stop=(k == 7))
                    s_ = sig[si[0] % 2]
                    pa = prodA[si[0] % 2]
                    si[0] += 1
                    P.tt("dve", s_, gb, rs_bc[:, hs], ALU.mult)
                    P.act(s_, s_, AF.Sigmoid)
                    if br == 0:
                        P.tt("dve", mergedT[:, dg * 4 + m, hs], bb, s_, ALU.mult)
                    else:
                        P.tt("dve", pa, bb, s_, ALU.mult)
                        P.tt("dve", mergedT[:, dg * 4 + m, hs], mergedT[:, dg * 4 + m, hs], pa, ALU.add)
    P.barrier()
    if T.get('stop') == 4:
        return

    at(20)
    hacc = P.alloc("hacc", (LB, 2048), F32)
    xst = [P.alloc("xst%d" % i, (KC, 128), F32) for i in range(2)]
    at(132)
    wos = [P.alloc("wo%d" % i, (KC, 512), BF16) for i in range(2)]
    xl = [P.alloc("xl%d" % i, (512,), F32) for i in range(2)]
    for dg in range(4):
        wo = wos[dg % 2]
        for k0 in range(0, KC, 4):
            s_ = xst[(k0 // 4) % 2].re("p k t -> p (k t)").re("p (a b) -> p a b", a=4)
            P.dma(s_, T["w_out"][:, k0:k0 + 4, dg * 512:(dg + 1) * 512])
            P.copy(eng2(), wo[:, k0:k0 + 4, :], s_)
        for j in range(LB):
            bk = P.bank(bank_rr[0] % 4)
            bank_rr[0] += 1
            for k in range(KC):
                P.mm(bk, mergedT[:, k, j * 128:(j + 1) * 128], wo[:, k, :], start=(k == 0), stop=(k == KC - 1))
            x_ = xl[(dg * LB + j) % 2]
            P.dma(x_, x_loc[j][:, dg * 512:(dg + 1) * 512])
            P.tt("dve", hacc[:, j, dg * 512:(dg + 1) * 512], bk, x_, ALU.add)
    if dbg:
        P.dma(dbg["h"], hacc)
    P.barrier()
    if T.get('stop') == 5:
        return

    at(116)
    ohf = P.alloc("ohf", (LB, 8), F32)
    ohb = P.alloc("ohb", (LB, 8), BF16)
    posf = P.alloc("posf", (LB, 8), F32)
    combb = P.alloc("combb", (LB, 64), BF16)
    triS_b = P.alloc("triS_b", (128,), BF16)
    iota256 = P.alloc("iota256", (256,), F32)
    combS = P.alloc("combS", (2, 8), F32)
    assert P.top <= KB(120), P.top
    at(84)
    x2tm = P.alloc("x2tm", (LB, 2048), BF16)
    at(120)
    x2Tb = [P.alloc("x2Tb%d" % i, (KC, 128), BF16) for i in range(2)]
    wr = P.alloc("wr", (KC, 72), BF16)
    wrs = P.alloc("wrs", (KC, 72), F32)
    br_bc = P.alloc("br_bc", (72,), F32)
    rt = [dict((n, P.alloc("r%s%d" % (n, i), sh, F32)) for n, sh in
               (("s2", (1,)), ("sd", (1,)), ("rs", (1,)), ("lg", (72,)), ("gm", (1,)), ("ge", (8,)), ("gs", (1,)),
                ("gv", (1,)), ("el", (8, 8)), ("sel", (8,)), ("m1", (1,)), ("k1", (8,)), ("s2l", (8,)),
                ("m2", (1,)), ("k2", (8,)), ("dd", (1,)), ("q1", (1,)), ("q2", (1,)), ("wn", (8,)),
                ("ohv", (8,)))) for i in range(2)]
    hj = P.alloc("hjunk", (2048,), F32)
    trS_f = P.alloc("trS_f", (128,), F32)
    P.dma(wrs, T["wr_d"])
    P.copy("pool", wr, wrs)
    P.dma(br_bc, bcast(T["br_d"]))
    P.tt("pool", trS_f, tri, ident_f, ALU.subtract)
    P.copy("pool", triS_b, trS_f)
    P.iota(iota256, [[1, 256]], 0, 0)
    for j in range(LB):
        i = j % 2
        r = rt[i]
        oh = ohf[:, j, :]
        P.act(hj, hacc[:, j, :], AF.Square, accum=r["s2"])
        P.act(r["sd"], r["s2"], AF.Sqrt, scale=1.0 / D, bias=epsb)
        P.recip(r["rs"], r["sd"])
        P.act(x2tm[:, j, :], hacc[:, j, :], AF.Identity, scale=r["rs"])
        for kq in range(4):
            tp = P.bank(4 + (j * 4 + kq) % 2, F32, 512)
            for kk in range(4):
                k = kq * 4 + kk
                P.tr(tp[:, kk * 128:(kk + 1) * 128], x2tm[:, j, k * 128:(k + 1) * 128], ident_b)
            for kk in range(4):
                k = kq * 4 + kk
                P.act(x2Tb[i][:, k, :], tp[:, kk * 128:(kk + 1) * 128], AF.Identity, scale=g2[:, k:k + 1])
        lb = P.bank(j % 2)
        for k in range(KC):
            P.mm(lb[:, 0:72], x2Tb[i][:, k, :], wr[:, k, :], start=(k == 0), stop=(k == KC - 1))
        P.tt("dve", r["lg"], lb[:, 0:72], br_bc, ALU.add)
        P.reduce(r["gm"], r["lg"][:, 0:8], ALU.max)
        P.ts("dve", oh, r["lg"][:, 0:8], r["gm"], ALU.is_equal)
        P.copy("act", ohb[:, j, :], oh)
        P.ts("dve", r["ge"], r["lg"][:, 0:8], r["gm"], ALU.subtract)
        P.act(r["ge"], r["ge"], AF.Exp, accum=r["gs"])
        P.recip(r["gv"], r["gs"])
        P.tt("dve", r["el"], r["lg"][:, 8:72].re("p (g e) -> p g e", g=8),
             oh.re("p (g o) -> p g o", o=1).bc([128, 8, 8]), ALU.mult)
        P.reduce(r["sel"], r["el"].re("p g e -> p e g"), ALU.add)
        P.reduce(r["m1"], r["sel"], ALU.max)
        P.ts("dve", r["k1"], r["sel"], r["m1"], ALU.is_equal)
        P.stt(r["s2l"], r["k1"], NEG, r["sel"], ALU.mult, ALU.add)
        P.reduce(r["m2"], r["s2l"], ALU.max)
        P.ts("dve", r["k2"], r["s2l"], r["m2"], ALU.is_equal)
        P.tt("dve", r["dd"], r["m2"], r["m1"], ALU.subtract)
        P.act(r["dd"], r["dd"], AF.Exp)
        P.ts("dve", r["q1"], r["dd"], 1.0, ALU.add)
        P.recip(r["q1"], r["q1"])
        P.tt("dve", r["q2"], r["dd"], r["q1"], ALU.mult)
        P.ts("dve", r["wn"], r["k1"], r["q1"], ALU.mult)
        P.stt(r["wn"], r["k2"], r["q2"], r["wn"], ALU.mult, ALU.add)
        P.ts("dve", r["ohv"], oh, r["gv"], ALU.mult)
        for g in range(8):
            P.ts("dve", comb[:, j, g * 8:(g + 1) * 8], r["wn"], r["ohv"][:, g:g + 1], ALU.mult)
    P.copy("act", combb, comb)
    for j in range(LB):
        ps = P.bank(2 + j % 2)
        P.mm(ps[:, 0:8], triS_b, ohb[:, j, :], start=True, stop=(j == 0))
        for j2 in range(j):
            P.mm(ps[:, 0:8], ones128, ohb[:, j2, :], start=False, stop=(j2 == j - 1))
        P.copy("act", posf[:, j, :], ps[:, 0:8])
    if dbg:
        P.dma(dbg["comb"], comb)
    P.barrier()
    if T.get('stop') == 6:
        return

    CAP = 256
    at(120)
    wg_h = P.alloc("wg_h", (KC, 256), BF16)
    wu_h = P.alloc("wu_h", (KC, 256), BF16)
    wd_b = P.alloc("wd_b", (4, 2048), BF16)
    est = [P.alloc("est%d" % i, (8, 256), F32) for i in range(3)]
    xy = P.alloc("xy", (KC, CAP), BF16)
    x2gT = xy
    ygb = V(xy.b, xy.ap.rearrange("p k s -> p (k s)").rearrange("p (a b) -> p a b", a=2))
    yg = P.alloc("yg", (2, 2048), F32)
    hidT = P.alloc("hidT", (4, CAP), BF16)
    sgt = [P.alloc("sgt%d" % i, (CAP,), BF16) for i in range(2)]
    assert P.top <= KB(207), P.top
    P.top = off_wuk
    Pg = P.alloc("Pg", (LB, CAP), BF16)
    PgT = P.alloc("PgT", (2, 1024), BF16)
    ei = 0
    cast_rot = ("act", "dve", "act", "dve")
    weg, weu, wed = T["weg"], T["weu"], T["wed"]
    n_exp_total = (T.get("n_experts", NE) // 8) * 8
    eic = [0]

    def moe_stage():
        s_ = est[eic[0] % 3]
        ce = cast_rot[eic[0] % 4]
        eic[0] += 1
        return s_, ce

    def moe_load_half(e, half):
        for (dst, srcw) in ((wg_h, weg), (wu_h, weu)):
            for k0 in (0, 8):
                s_, ce = moe_stage()
                P.dma(s_, srcw[e][half][:, k0:k0 + 8, :])
                P.copy(ce, dst[:, k0:k0 + 8, :], s_)

    def moe_load_wd(e):
        for kf in range(4):
            s_, ce = moe_stage()
            s_ = s_.re("p a b -> p (a b)")
            P.dma(s_, wed[e][:, kf, :])
            P.copy(ce, wd_b[:, kf, :], s_)

    for g in range(T.get("n_experts", NE) // 8):
        for j in range(LB):
            P.ts("dve", Pg[:, j, :], iota256, posf[:, j, g:g + 1], ALU.is_equal, ohf[:, j, g:g + 1], ALU.mult)
        for sb in range(2):
            for jq in range(2):
                tp = P.bank(6 + jq)
                for jj in range(4):
                    j = jq * 4 + jj
                    P.tr(tp[:, jj * 128:(jj + 1) * 128], Pg[:, j, sb * 128:(sb + 1) * 128], ident_b)
                P.copy("act", PgT[:, sb, jq * 512:(jq + 1) * 512], tp)
        for sb in range(2):
            cp = P.bank(4 + sb)
            for j in range(LB):
                P.mm(cp[:, 0:8], Pg[:, j, sb * 128:(sb + 1) * 128], combb[:, j, g * 8:(g + 1) * 8],
                     start=(j == 0), stop=(j == LB - 1))
            P.copy("act", combS[:, sb, :], cp[:, 0:8])
        for k in range(KC):
            xp = P.bank(k % 4)
            for j in range(LB):
                P.mm(xp[:, 0:CAP], x2tm[:, j, k * 128:(k + 1) * 128], Pg[:, j, :], start=(j == 0), stop=(j == LB - 1))
            P.act(x2gT[:, k, :], xp[:, 0:CAP], AF.Identity, scale=g2[:, k:k + 1])
        for el in range(8):
            e = g * 8 + el
            if e == 0:
                moe_load_half(0, 0)
            moe_load_wd(e)
            for half in range(2):
                if half == 1:
                    moe_load_half(e, 1)
                for fcl in range(2):
                    fc = half * 2 + fcl
                    gb = P.bank(fcl)
                    ub = P.bank(2 + fcl)
                    for k in range(KC):
                        P.mm(gb[:, 0:CAP], wg_h[:, k, fcl * 128:(fcl + 1) * 128], x2gT[:, k, :],
                             start=(k == 0), stop=(k == KC - 1))
                    for k in range(KC):
                        P.mm(ub[:, 0:CAP], wu_h[:, k, fcl * 128:(fcl + 1) * 128], x2gT[:, k, :],
                             start=(k == 0), stop=(k == KC - 1))
                    sg_ = sgt[fc % 2]
                    P.act(sg_, gb[:, 0:CAP], AF.Silu)
                    P.tt("dve", hidT[:, fc, :], ub[:, 0:CAP], sg_, ALU.mult)
            if e + 1 < n_exp_total:
                moe_load_half(e + 1, 0)
            for sb in range(2):
                for dq in range(4):
                    yb = P.bank(4 + dq % 2)
                    for kf in range(4):
                        P.mm(yb, hidT[:, kf, sb * 128:(sb + 1) * 128], wd_b[:, kf, dq * 512:(dq + 1) * 512],
                             start=(kf == 0), stop=(kf == 3))
                    ysl = yg[:, sb, dq * 512:(dq + 1) * 512]
                    if el == 0:
                        P.ts("dve", ysl, yb, combS[:, sb, el:el + 1], ALU.mult)
                    else:
                        P.stt(ysl, yb, combS[:, sb, el:el + 1], ysl, ALU.mult, ALU.add)
        P.copy("act", ygb, yg)
        for j in range(LB):
            for dq in range(4):
                ps = P.bank(6 + dq % 2)
                for sb in range(2):
                    P.mm(ps, PgT[:, sb, j * 128:(j + 1) * 128], ygb[:, sb, dq * 512:(dq + 1) * 512],
                         start=(sb == 0), stop=(sb == 1))
                hsl = hacc[:, j, dq * 512:(dq + 1) * 512]
                P.tt("dve", hsl, ps, hsl, ALU.add)
    P.barrier()
    if T.get('stop') == 7:
        return

    at(116)
    gf_bc = P.alloc("gf_bc", (2048,), F32)
    fj = P.alloc("fjunk", (2048,), F32)
    fo = [P.alloc("fo%d" % i, (2048,), F32) for i in range(2)]
    fs = [dict((n, P.alloc("f%s%d" % (n, i), (1,), F32)) for n in ("s2", "sd", "rs")) for i in range(2)]
    P.dma(gf_bc, bcast(T["gf_d"]))
    for j in range(LB):
        i = j % 2
        f = fs[i]
        P.act(fj, hacc[:, j, :], AF.Square, accum=f["s2"])
        P.act(f["sd"], f["s2"], AF.Sqrt, scale=1.0 / D, bias=epsb)
        P.recip(f["rs"], f["sd"])
        P.stt(fo[i], hacc[:, j, :], f["rs"], gf_bc, ALU.mult, ALU.mult)
        P.dma(out_d[j], fo[i])
    P.barrier()
    if T.get('stop') == 8:
        return


_NC_CACHE = {}


def _layout_inputs(inp, n_experts=NE):
    f = lambda a: np.ascontiguousarray(np.asarray(a, dtype=np.float32))
    x = f(inp["x"]).reshape(S, D)
    xT = np.ascontiguousarray(x.reshape(NB, 128, KC, 128).transpose(0, 3, 2, 1)).reshape(NB, 128, 2048)
    xb = x.reshape(NB, 128, D)
    pk = lambda v: np.ascontiguousarray(f(v).reshape(-1, 128).T)
    pkm = lambda w: np.ascontiguousarray(f(w).reshape(-1, 128, w.shape[-1]).transpose(1, 0, 2))
    shared = {
        "xT_all": xT,
        "g1": pk(inp["norm1_g"]),
        "w_in": pkm(inp["w_in"]),
        "gkv": f(inp["kv_norm_g"]),
        "w_uk": f(inp["w_uk"]),
        "w_uv": f(inp["w_uv"]),
        "wsT": np.ascontiguousarray(f(inp["gmlp_ws"]).transpose(0, 2, 1)),
        "bs": f(inp["gmlp_bs"]).reshape(1024),
        "lng": f(inp["ln_v_g"]),
        "lnb": f(inp["ln_v_b"]),
        "w_bra": pkm(inp["w_br_attn"]),
        "w_brg": pkm(inp["w_br_gmlp"]),
        "w_out": pkm(inp["w_out"]),
        "g2": pk(inp["norm2_g"]),
        "wr": pkm(np.concatenate([f(inp["w_group"]), f(inp["w_router"])], axis=1)),
        "br": np.concatenate([f(inp["b_group"]), f(inp["b_router"])]),
        "weg": np.ascontiguousarray(f(inp["w_e_gate"][:n_experts]).reshape(n_experts, KC, 128, 2, 256).transpose(0, 3, 2, 1, 4)),
        "weu": np.ascontiguousarray(f(inp["w_e_up"][:n_experts]).reshape(n_experts, KC, 128, 2, 256).transpose(0, 3, 2, 1, 4)),
        "wed": np.ascontiguousarray(f(inp["w_e_down"][:n_experts]).reshape(n_experts, 4, 128, D).transpose(0, 2, 1, 3)),
        "gf": f(inp["norm_f_g"]),
    }
    maps = []
    for c in range(NCORES):
        blks = [8 * j + c for j in range(LB)]
        m = dict(shared)
        m["xT_loc"] = np.ascontiguousarray(xT[blks])
        m["x_loc"] = np.ascontiguousarray(xb[blks])
        m["trel"] = (c * 128 + np.arange(128, dtype=np.float32)).reshape(128, 1)
        maps.append(m)
    return maps


def kernel(**inputs):
    if "nc" not in _NC_CACHE:
        _NC_CACHE["nc"] = build(False)
    nc = _NC_CACHE["nc"]
    maps = _layout_inputs(inputs)
    res = run_bass_kernel_spmd(nc, maps, core_ids=list(range(NCORES)))
    out = np.zeros((NB, 128, D), np.float32)
    for c in range(NCORES):
        o = np.asarray(res.results[c]["out"]).reshape(LB, 128, D)
        for j in range(LB):
            out[8 * j + c] = o[j]
    return out.reshape(1, S, D)
```

```python
import numpy as np
import concourse.bass as bass
import concourse.mybir as mybir
from concourse.bass_utils import run_bass_kernel_spmd

F32 = mybir.dt.float32
BF16 = mybir.dt.bfloat16
AF = mybir.ActivationFunctionType
ALU = mybir.AluOpType
AX = mybir.AxisListType

NCORES = 8
S = 8192
D = 2048
NB = 64
LB = 8
KC = 16
IN_COLS = 8528
C_Q, C_KV, C_QI, C_KI, C_WI, C_UV, C_G = 0, 1024, 1280, 2304, 2368, 2384, 4432
EPS = 1e-6
TOPK = 256
NE = 64
FF = 512
BIS_R = 32.0
BIS_N = 24
NEG = -1.0e30


class Ev:
    __slots__ = ("kind", "eng", "rec", "sem", "val")

    def __init__(self, kind, eng=None, rec=None, sem=None, val=None):
        self.kind, self.eng, self.rec, self.sem, self.val = kind, eng, rec, sem, val


class Buf:
    def __init__(self, name):
        self.name = name
        self.w = None
        self.r = {}
        self.sem = None
        self.semval = 0


def _evkey(ev):
    return ("e", ev.eng) if ev.kind == "eng" else ("d", id(ev.sem))


def _evrank(ev):
    return ev.rec["idx"] if ev.kind == "eng" else ev.val


class V:
    def __init__(self, b, ap):
        self.b, self.ap = b, ap

    def __getitem__(self, idx):
        return V(self.b, self.ap[idx])

    def re(self, pat, **kw):
        return V(self.b, self.ap.rearrange(pat, **kw))

    def bc(self, shape):
        return V(self.b, self.ap.broadcast_to(shape))

    def cast(self, dt):
        return V(self.b, self.ap.bitcast(dt))


ENGS = ("pe", "act", "dve", "pool", "sp")


class Prog:
    def __init__(self, nc, arena, psum, sems):
        self.nc = nc
        self.arena = arena
        self.arena_words = arena.shape[1]
        self.top = 0
        self.psum = psum
        self.banks = [Buf("bank%d" % i) for i in range(8)]
        for b_ in self.banks:
            b_.excl = True
        self.streams = {e: [] for e in ENGS}
        self.free_sems = list(sems)
        self.eng_sem = {e: self.free_sems.pop() for e in ENGS}
        self.dma_events = {}

    def alloc(self, name, free_shape, dt):
        nelem = int(np.prod(free_shape))
        bpe = 4 if dt == F32 else 2
        words = (nelem * bpe + 3) // 4
        words = (words + 15) // 16 * 16
        assert self.top + words <= self.arena_words, ("SBUF arena overflow", name, self.top, words)
        ap = self.arena[:, self.top:self.top + words]
        self.top += words
        if dt != F32:
            ap = ap.bitcast(dt)
        ap = ap[:, 0:nelem]
        if len(free_shape) == 2:
            ap = ap.rearrange("p (a b) -> p a b", a=free_shape[0])
        elif len(free_shape) == 3:
            ap = ap.rearrange("p (a b c) -> p a b c", a=free_shape[0], b=free_shape[1])
        elif len(free_shape) == 4:
            ap = ap.rearrange("p (a b c d) -> p a b c d", a=free_shape[0], b=free_shape[1], c=free_shape[2])
        return V(Buf(name), ap)

    def bank(self, i, dt=F32, cols=512):
        assert dt == F32 and i < 8
        ap = self.psum[:, i * 512:(i + 1) * 512]
        return V(self.banks[i], ap[:, 0:cols])

    def tbank(self, i, cols=512):
        ap = self.psum_b[:, i * 1024:(i + 1) * 1024]
        return V(self.banks[6 + i], ap[:, 0:cols])

    def _rec(self, eng, fn, reads, writes, dma=False):
        rec = {"fn": fn, "waits": [], "sig": False, "cnt": None, "dma": None, "idx": len(self.streams[eng])}
        is_pe = eng == "pe"
        best = {}

        def need(ev, raw):
            if ev.kind == "eng" and ev.eng == eng and not dma:
                if is_pe or not raw:
                    return
            k = _evkey(ev)
            if k not in best or _evrank(best[k]) < _evrank(ev):
                best[k] = ev

        for v in reads:
            b = v.b if isinstance(v, V) else v
            if b.w is not None:
                need(b.w, True)
            if getattr(b, "excl", False):
                for e_ in b.r.values():
                    need(e_, False)
        for v in writes:
            b = v.b if isinstance(v, V) else v
            if b.w is not None:
                need(b.w, False)
            for e_ in b.r.values():
                need(e_, False)
        for ev in best.values():
            rec["waits"].append(ev)
            if ev.kind == "eng":
                ev.rec["sig"] = True
        if dma:
            owner = None
            for v in list(writes) + list(reads):
                b = v.b if isinstance(v, V) else v
                if not getattr(b, "is_dram", False):
                    owner = b
                    break
            assert owner is not None
            if owner.sem is None:
                owner.sem = self.free_sems.pop()
            owner.semval += 16
            ev = Ev("dma", sem=owner.sem, val=owner.semval)
            rec["dma"] = ev
            k = _evkey(ev)
            self.dma_events[k] = ev
        else:
            ev = Ev("eng", eng=eng, rec=rec)
        for v in reads:
            b = v.b if isinstance(v, V) else v
            b.r[_evkey(ev)] = ev
        for v in writes:
            b = v.b if isinstance(v, V) else v
            b.w = ev
            b.r = {}
        self.streams[eng].append(rec)
        return ev

    def barrier(self):
        lasts = {}
        for e in ENGS:
            for rec in reversed(self.streams[e]):
                if rec["dma"] is None and rec["fn"] is not None:
                    lasts[e] = Ev("eng", eng=e, rec=rec)
                    break
        dmas = list(self.dma_events.values())
        self.dma_events = {}
        for e in ENGS:
            rec = {"fn": None, "waits": [], "sig": False, "cnt": None, "dma": None, "idx": len(self.streams[e])}
            for f, ev in lasts.items():
                if f != e:
                    rec["waits"].append(ev)
                    ev.rec["sig"] = True
            rec["waits"].extend(dmas)
            self.streams[e].append(rec)

    def emit(self, eng, e):
        sem_e = self.eng_sem[eng]
        waited = {}
        for rec in self.streams[eng]:
            for ev in rec["waits"]:
                if ev.kind == "eng":
                    sem, val = self.eng_sem[ev.eng], ev.rec["cnt"]
                else:
                    sem, val = ev.sem, ev.val
                key = id(sem)
                if waited.get(key, 0) >= val:
                    continue
                waited[key] = val
                e.wait_ge(sem, val)
            if rec["fn"] is None:
                continue
            ins = rec["fn"](e)
            if rec["dma"] is not None:
                ins.then_inc(rec["dma"].sem, 16)
            elif rec["sig"]:
                ins.then_inc(sem_e, 1)

    def finalize(self):
        for eng in ENGS:
            n = 0
            for rec in self.streams[eng]:
                if rec["sig"]:
                    n += 1
                    rec["cnt"] = n

    @staticmethod
    def _a(x):
        return x.ap if isinstance(x, V) else x

    def mm(self, out, lhsT, rhs, start=True, stop=True):
        o, l, r = out.ap, lhsT.ap, rhs.ap
        self._rec("pe", lambda e: e.matmul(o, l, r, start=start, stop=stop), [lhsT, rhs], [out])

    def tr(self, out, in_, ident):
        o, i, d = out.ap, in_.ap, ident.ap
        self._rec("pe", lambda e: e.matmul(o, i, d, start=True, stop=True), [in_, ident], [out])

    def act(self, out, in_, func, scale=1.0, bias=None, accum=None):
        o, i = out.ap, in_.ap
        reads = [in_]
        writes = [out]
        kw = {}
        if isinstance(scale, V):
            reads.append(scale)
        kw["scale"] = self._a(scale)
        if bias is not None:
            if isinstance(bias, V):
                reads.append(bias)
            kw["bias"] = self._a(bias)
        if accum is not None:
            writes.append(accum)
            kw["accum_out"] = accum.ap
        self._rec("act", lambda e: e.activation(o, i, func, **kw), reads, writes)

    def ts(self, eng, out, in0, s1, op0, s2=None, op1=None, accum=None):
        o, i = out.ap, in0.ap
        reads = [in0] + [s for s in (s1, s2) if isinstance(s, V)]
        writes = [out]
        a1, a2 = self._a(s1), self._a(s2)
        kw = {}
        if op1 is not None:
            kw["op1"] = op1
        if accum is not None:
            writes.append(accum)
            kw["accum_out"] = accum.ap
        self._rec(eng, lambda e: e.tensor_scalar(o, i, a1, a2, op0, **kw), reads, writes)

    def tt(self, eng, out, in0, in1, op):
        o, a, b = out.ap, in0.ap, in1.ap
        self._rec(eng, lambda e: e.tensor_tensor(o, a, b, op), [in0, in1], [out])

    def stt(self, out, in0, scalar, in1, op0, op1):
        o, a, b = out.ap, in0.ap, in1.ap
        reads = [in0, in1] + ([scalar] if isinstance(scalar, V) else [])
        s = self._a(scalar)
        self._rec("dve", lambda e: e.scalar_tensor_tensor(o, a, s, b, op0, op1), reads, [out])

    def copy(self, eng, out, in_):
        o, i = out.ap, in_.ap
        if eng == "act":
            self._rec("act", lambda e: e.activation(o, i, AF.Identity), [in_], [out])
        else:
            self._rec(eng, lambda e: e.tensor_copy(o, i), [in_], [out])

    def recip(self, out, in_):
        o, i = out.ap, in_.ap
        self._rec("dve", lambda e: e.reciprocal(o, i), [in_], [out])

    def reduce(self, out, in_, op, axis=AX.X):
        o, i = out.ap, in_.ap
        self._rec("dve", lambda e: e.tensor_reduce(o, i, axis, op), [in_], [out])

    def memset(self, eng, out, val):
        o = out.ap
        self._rec(eng, lambda e: e.memset(o, val), [], [out])

    def iota(self, out, pattern, base, cm):
        o = out.ap
        self._rec("pool", lambda e: e.iota(o, pattern, base=base, channel_multiplier=cm,
                                           allow_small_or_imprecise_dtypes=True), [], [out])

    def dma(self, out, in_, q="sp"):
        o, i = out.ap, in_.ap
        return self._rec(q, lambda e: e.dma_start(out=o, in_=i), [in_], [out], dma=True)


class DBuf(Buf):
    is_dram = True


def dram_v(nc, name, shape, dt, kind):
    t = nc.dram_tensor(name, list(shape), dt, kind=kind)
    return V(DBuf(name), t.ap())


def build(debug=False, n_experts=NE, stop=None, nb_p0=None):
    nc = bass.Bass("TRN2", target_bir_lowering=False)
    I = lambda n, s, dt=F32: dram_v(nc, n, s, dt, "ExternalInput")
    xT_all = I("xT_all", [NB, 128, 2048])
    xT_loc = I("xT_loc", [LB, 128, 2048])
    x_loc = I("x_loc", [LB, 128, 2048])
    trel_d = I("trel", [128, 1])
    g1_d = I("g1", [128, KC])
    w_in = I("w_in", [128, KC, IN_COLS])
    gkv_d = I("gkv", [256])
    w_uk = I("w_uk", [8, 128, 256])
    w_uv = I("w_uv", [8, 256, 128])
    wsT_d = I("wsT", [8, 128, 128])
    bs_d = I("bs", [1024])
    lng_d = I("lng", [1024])
    lnb_d = I("lnb", [1024])
    w_bra = I("w_bra", [128, 8, 2048])
    w_brg = I("w_brg", [128, 8, 2048])
    w_out = I("w_out", [128, KC, 2048])
    g2_d = I("g2", [128, KC])
    wr_d = I("wr", [128, KC, 72])
    br_d = I("br", [72])
    weg = I("weg", [n_experts, 2, 128, KC, 256])
    weu = I("weu", [n_experts, 2, 128, KC, 256])
    wed = I("wed", [n_experts, 128, 4, 2048])
    gf_d = I("gf", [2048])
    out_d = dram_v(nc, "out", [LB, 128, 2048], F32, "ExternalOutput")
    ckv_s = dram_v(nc, "ckv_s", [NB, 128, 256], BF16, "Internal")
    qT_s = dram_v(nc, "qT_s", [LB, 128, 1024], BF16, "Internal")
    qiT_s = dram_v(nc, "qiT_s", [LB, 128, 1024], BF16, "Internal")
    dbg = {}
    if debug:
        dbg["ckvT"] = dram_v(nc, "dbg_ckvT", [128, 2, S], BF16, "ExternalOutput")
        dbg["kT"] = dram_v(nc, "dbg_kT", [128, S], BF16, "ExternalOutput")
        dbg["aT"] = dram_v(nc, "dbg_aT", [128, 8, 1024], BF16, "ExternalOutput")
        dbg["mT"] = dram_v(nc, "dbg_mT", [128, 8, 1024], BF16, "ExternalOutput")
        dbg["h"] = dram_v(nc, "dbg_h", [128, LB, 2048], F32, "ExternalOutput")
        dbg["thr"] = dram_v(nc, "dbg_thr", [128, LB], F32, "ExternalOutput")
        dbg["comb"] = dram_v(nc, "dbg_comb", [128, LB, 64], F32, "ExternalOutput")

    import contextlib
    with contextlib.ExitStack() as st:
        arena = st.enter_context(nc.sbuf_tensor("arena", [128, 207 * 256], F32))
        psum = st.enter_context(nc.psum_tensor("psum", [128, 4096], F32))
        sems = [st.enter_context(nc.semaphore("s%d" % i)) for i in range(96)]
        P = Prog(nc, arena, psum, sems)
        program(P, locals())
        P.finalize()
        block = st.enter_context(nc.Block())

        @block.tensor
        def _(e):
            P.emit("pe", e)

        @block.scalar
        def _(e):
            P.emit("act", e)

        @block.vector
        def _(e):
            P.emit("dve", e)

        @block.gpsimd
        def _(e):
            P.emit("pool", e)

        @block.sync
        def _(e):
            P.emit("sp", e)
    return nc


def program(P, T):
    xT_all, xT_loc, x_loc, w_in = T["xT_all"], T["xT_loc"], T["x_loc"], T["w_in"]
    out_d, dbg = T["out_d"], T["dbg"]
    blocks = lambda v, n: [V(DBuf("%s%d" % (v.b.name, i)), v.ap[i]) for i in range(n)]
    ckv_s = blocks(T["ckv_s"], NB)
    qT_s = blocks(T["qT_s"], LB)
    qiT_s = blocks(T["qiT_s"], LB)
    KB = lambda kb: kb * 256
    rr = [0]

    def eng2():
        rr[0] ^= 1
        return "act" if rr[0] else "dve"

    def at(kb):
        P.top = KB(kb)

    def rot(lst):
        c = [0]

        def f():
            c[0] += 1
            return lst[(c[0] - 1) % len(lst)]
        return f

    def bcast(v):
        return V(v.b, v.ap.partition_broadcast(128))

    ident_f = P.alloc("ident_f", (128,), F32)
    ident_b = P.alloc("ident_b", (128,), BF16)
    ones128 = P.alloc("ones128", (128,), BF16)
    iot = P.alloc("iot", (128,), F32)
    tri = P.alloc("tri", (128,), F32)
    g1 = P.alloc("g1", (KC,), F32)
    g2 = P.alloc("g2", (KC,), F32)
    trel = P.alloc("trel", (1,), F32)
    epsb = P.alloc("epsb", (1,), F32)
    gkv_bc = P.alloc("gkv_bc", (256,), F32)
    wi = P.alloc("wi", (LB, 16), F32)
    rs_tm = P.alloc("rs_tm", (LB,), F32)
    rs_bc = P.alloc("rs_bc", (1024,), F32)
    comb = P.alloc("comb", (LB, 64), F32)
    thr = P.alloc("thr", (LB,), F32)
    off_wuk = P.top
    wuk = P.alloc("wuk", (8, 256), BF16)
    wuv = P.alloc("wuv", (8, 2, 128), BF16)
    assert P.top <= KB(20), P.top
    P.iota(iot, [[1, 128]], 0, -1)
    P.ts("pool", ident_f, iot, 0.0, ALU.is_equal)
    P.copy("pool", ident_b, ident_f)
    P.memset("pool", ones128, 1.0)
    P.memset("pool", epsb, EPS)
    P.ts("pool", tri, iot, 0.0, ALU.is_ge)
    P.dma(g1, T["g1_d"])
    P.dma(g2, T["g2_d"])
    P.dma(trel, T["trel_d"])
    P.dma(gkv_bc, bcast(T["gkv_d"]))
    if T.get('stop') == -1:
        P.barrier()
        return
    at(20)
    aT = P.alloc("aT", (8, 1024), BF16)
    ckvT = P.alloc("ckvT", (2, S), BF16)
    kT = P.alloc("kT", (S,), BF16)
    assert P.top == KB(84), P.top

    def load_w_cols(dst, c0, c1, stg2, fold=True):
        n = c1 - c0
        for k0 in range(0, KC, 4):
            stg = stg2[(k0 // 4) % 2].re("p k t -> p (k t)").re("p (a b) -> p a b", a=4)
            P.dma(stg[:, :, 0:n], w_in[:, k0:k0 + 4, c0:c1])
            for k in range(k0, k0 + 4):
                if eng2() == "act":
                    P.act(dst[:, k, 0:n], stg[:, k - k0, 0:n], AF.Identity, scale=g1[:, k:k + 1])
                else:
                    P.ts("dve", dst[:, k, 0:n], stg[:, k - k0, 0:n], g1[:, k:k + 1], ALU.mult)

    at(84)
    xst = [P.alloc("xst%d" % i, (KC, 128), F32) for i in range(2)]
    wkv = P.alloc("wkv", (KC, 320), BF16)
    load_w_cols(wkv[:, :, 0:256], C_KV, C_KV + 256, xst)
    load_w_cols(wkv[:, :, 256:320], C_KI, C_KI + 64, xst)
    if T.get('stop') == -2:
        P.barrier()
        return
    xb = [P.alloc("xb%d" % i, (KC, 128), BF16) for i in range(2)]
    xsq = [P.alloc("xsq%d" % i, (KC, 128), BF16) for i in range(2)]
    sm = [dict((n, P.alloc("%s%d" % (n, i), (1,), F32)) for n in ("sd", "rs", "sv", "mv", "rv", "rr"))
          for i in range(2)]
    junk = [P.alloc("junk%d" % i, (256,), F32) for i in range(2)]
    ckv_tm = [P.alloc("ckv_tm%d" % i, (256,), BF16) for i in range(2)]
    k_tm = [P.alloc("k_tm%d" % i, (128,), BF16) for i in range(2)]
    for i in range(2):
        P.memset("pool", k_tm[i], 0.0)
    def p0_front(b):
        i = b % 2
        P.dma(xst[i].re("p k t -> p (k t)"), xT_all[b])
        P.copy("dve", xb[i], xst[i])
        P.act(xsq[i], xst[i], AF.Square)
        kv = P.bank(i)
        ss = P.bank(2 + i)
        for k in range(KC):
            P.mm(kv[:, 0:320], xb[i][:, k, :], wkv[:, k, :], start=(k == 0), stop=(k == KC - 1))
        for k in range(KC):
            P.mm(ss[:, 0:1], xsq[i][:, k, :], ones128[:, 0:1], start=(k == 0), stop=(k == KC - 1))

    def p0_back(b):
        i = b % 2
        kv = P.bank(i)
        ss = P.bank(2 + i)
        s = sm[i]
        P.act(s["sd"], ss[:, 0:1], AF.Sqrt, scale=1.0 / D, bias=epsb)
        P.recip(s["rs"], s["sd"])
        P.act(junk[i], kv[:, 0:256], AF.Square, accum=s["sv"])
        P.ts("dve", s["mv"], s["sv"], s["rs"], ALU.mult, s["rs"], ALU.mult)
        P.act(s["rv"], s["mv"], AF.Sqrt, scale=1.0 / 256, bias=epsb)
        P.recip(s["rr"], s["rv"])
        P.tt("dve", s["rr"], s["rr"], s["rs"], ALU.mult)
        P.stt(ckv_tm[i], kv[:, 0:256], s["rr"], gkv_bc, ALU.mult, ALU.mult)
        P.act(k_tm[i][:, 0:64], kv[:, 256:320], AF.Identity, scale=s["rs"])
        P.dma(ckv_s[b], ckv_tm[i])
        tp = P.bank(4 + i, F32, 384)
        P.tr(tp[:, 0:128], ckv_tm[i][:, 0:128], ident_b)
        P.tr(tp[:, 128:256], ckv_tm[i][:, 128:256], ident_b)
        P.tr(tp[:, 256:384], k_tm[i], ident_b)
        P.copy("dve", ckvT[:, :, b * 128:(b + 1) * 128], tp[:, 0:256].re("p (c t) -> p c t", c=2))
        P.copy("act", kT[0:64, b * 128:(b + 1) * 128], tp[0:64, 256:384])

    nb0 = T.get('nb_p0') or NB
    p0_front(0)
    for b in range(nb0):
        if b + 1 < nb0:
            p0_front(b + 1)
        p0_back(b)
    if T.get('stop') in (-3, -4, -5):
        P.barrier()
        return
    P.dma(kT[64:128, :], kT[0:64, :])
    if dbg:
        P.dma(dbg["ckvT"], ckvT)
        P.dma(dbg["kT"], kT)
    P.barrier()
    if T.get('stop') == 0:
        return

    bank_rr = [0]

    def load_local_x(xTl, xst, stats):
        xsq1 = [P.alloc("xsq1_%d" % i, (KC, 128), BF16) for i in range(2)] if stats else None
        sd1 = [P.alloc("sd1_%d" % i, (1,), F32) for i in range(2)] if stats else None
        sdb = [P.alloc("sdb_%d" % i, (128,), F32) for i in range(2)] if stats else None
        for j in range(LB):
            i = j % 2
            P.dma(xst[i].re("p k t -> p (k t)"), xT_loc[j])
            P.copy("dve", xTl[:, :, j * 128:(j + 1) * 128], xst[i])
            if not stats:
                continue
            P.act(xsq1[i], xst[i], AF.Square)
            ss = P.bank(2 + i)
            for k in range(KC):
                P.mm(ss[:, 0:1], xsq1[i][:, k, :], ones128[:, 0:1], start=(k == 0), stop=(k == KC - 1))
            P.act(sd1[i], ss[:, 0:1], AF.Sqrt, scale=1.0 / D, bias=epsb)
            P.recip(rs_tm[:, j:j + 1], sd1[i])
            sb_ = P.bank(i)
            for k in range(KC):
                P.mm(sb_[:, 0:128], ones128, xsq1[i][:, k, :], start=(k == 0), stop=(k == KC - 1))
            P.act(sdb[i], sb_[:, 0:128], AF.Sqrt, scale=1.0 / D, bias=epsb)
            P.recip(rs_bc[:, j * 128:(j + 1) * 128], sdb[i])

    def fm_proj(wq, xTl, xst, c0, ncols, evac):
        load_w_cols(wq, c0, c0 + ncols, xst)
        for m in range(ncols // 128):
            for half in range(2):
                bk = P.bank(bank_rr[0] % 4)
                bank_rr[0] += 1
                for k in range(KC):
                    P.mm(bk, wq[:, k, m * 128:(m + 1) * 128], xTl[:, k, half * 512:(half + 1) * 512],
                         start=(k == 0), stop=(k == KC - 1))
                evac(m, half, bk)

    at(84)
    xTl = P.alloc("xTl", (KC, 1024), BF16)
    xst = [P.alloc("xst%d" % i, (KC, 128), F32) for i in range(2)]
    next_wq = rot([P.alloc("wq%d" % i, (KC, 512), BF16) for i in range(2)])
    qT_st = P.alloc("qT_st", (4, 1024), BF16)
    wwi = P.alloc("wwi", (KC, 16), BF16)
    load_local_x(xTl, xst, True)
    su = xst[0].re("p k t -> p (k t)").re("p (h c) -> p h c", h=8)
    P.dma(su, V(T["w_uk"].b, T["w_uk"].ap.rearrange("h d c -> d h c")))
    P.copy("pool", wuk, su)
    sv_ = xst[1].re("p k t -> p (k t)").re("p (h cc d) -> p h cc d", h=8, cc=2)
    for h in range(8):
        P.dma(sv_[:, h, :, :], V(T["w_uv"].b, T["w_uv"].ap[h].rearrange("(cc p) d -> p cc d", p=128)))
    P.copy("pool", wuv, sv_)
    for (c0, dst, sc) in ((C_Q, qT_s, 1.0), (C_QI, qiT_s, 0.125)):
        for grp in range(2):
            def evac(m, half, bk, sc=sc):
                P.stt(qT_st[:, m, half * 512:(half + 1) * 512], bk, sc, rs_bc[:, half * 512:(half + 1) * 512],
                      ALU.mult, ALU.mult)
            fm_proj(next_wq(), xTl, xst, c0 + grp * 512, 512, evac)
            for j in range(LB):
                P.dma(dst[j].re("p (m t) -> p m t", m=8)[:, grp * 4:(grp + 1) * 4, :],
                      qT_st[:, :, j * 128:(j + 1) * 128])
    load_w_cols(wwi, C_WI, C_WI + 16, xst)
    for j in range(LB):
        bk = P.bank(4 + j % 2)
        for k in range(KC):
            P.mm(bk[:, 0:16], xTl[:, k, j * 128:(j + 1) * 128], wwi[:, k, :], start=(k == 0), stop=(k == KC - 1))
        P.ts("dve", wi[:, j, :], bk[:, 0:16], rs_tm[:, j:j + 1], ALU.mult, 0.25, ALU.mult)
    P.barrier()
    if T.get('stop') == 1:
        return

    at(84)
    srel = P.alloc("srel", (1024,), F32)
    negm = P.alloc("negm", (1024,), F32)
    score = P.alloc("score", (S,), F32)
    maskT = P.alloc("maskT", (NB, 128), BF16)
    qT_b = P.alloc("qT_b", (8, 128), BF16)
    qiT_b = P.alloc("qiT_b", (8, 128), BF16)
    qa_b = P.alloc("qa_b", (2, 8, 128), BF16)
    Dh = P.alloc("Dh", (16, 128), BF16)
    Rb = [P.alloc("R%d" % i, (512,), BF16) for i in range(4)]
    PTb = [P.alloc("PT%d" % i, (4, 128), BF16) for i in range(3)]
    ckvb = [P.alloc("ckvb%d" % i, (256,), BF16) for i in range(4)]
    mk = [P.alloc("mk%d" % i, (512,), BF16) for i in range(2)]
    cnt = P.alloc("cnt", (1,), F32)
    mid = P.alloc("mid", (1,), F32)
    t1 = P.alloc("t1", (1,), F32)
    bjunk = P.alloc("bjunk", (S,), BF16)
    rZ = P.alloc("rZ", (512,), F32)
    OTn = P.alloc("OTn", (2, 4, 128), BF16)
    P.iota(srel, [[1, 1024]], 0, 0)
    P.ts("dve", negm, srel, trel[:, 0:1], ALU.is_gt, NEG, ALU.mult)
    SCALE = 128 ** -0.5
    cntr = {"ri": 0, "pti": 0, "cki": 0}
    rZ2 = [rZ, P.alloc("rZb", (512,), F32)]
    OTn2 = [OTn, P.alloc("OTnb", (2, 4, 128), BF16)]

    def p2_idx(j):
        nblk = 8 * (j + 1)
        P.dma(qiT_b.re("p m t -> p (m t)"), qiT_s[j])
        for h in range(16):
            P.ts("pool", Dh[:, h, :], ident_f, wi[:, j, h:h + 1], ALU.mult)
        for sg in range(nblk // 4):
            sc_ps = P.bank(4 + sg % 2)

            def emit_pair(p, sg=sg):
                Rs = []
                for hh in range(2):
                    dps = P.bank((p % 2) * 2 + hh)
                    lo = 64 * hh
                    P.mm(dps, qiT_b[lo:lo + 64, p, :], kT[lo:lo + 64, sg * 512:(sg + 1) * 512])
                    R = Rb[cntr["ri"] % 4]
                    cntr["ri"] += 1
                    if hh == 0:
                        P.act(R, dps, AF.Relu)
                    else:
                        P.ts("dve", R, dps, 0.0, ALU.max)
                    Rs.append(R)
                return Rs

            Rp = emit_pair(0)
            for p in range(8):
                Rn = emit_pair(p + 1) if p + 1 < 8 else None
                for hh in range(2):
                    h = 2 * p + hh
                    P.mm(sc_ps, Dh[:, h, :], Rp[hh], start=(h == 0), stop=(h == 15))
                Rp = Rn
            if sg >= 2 * j:
                o = (sg - 2 * j) * 512
                P.tt("dve", score[:, sg * 512:(sg + 1) * 512], sc_ps, negm[:, o:o + 512], ALU.add)
            else:
                P.copy("act", score[:, sg * 512:(sg + 1) * 512], sc_ps)

    def p2_bis(j):
        Sj = 8 * (j + 1) * 128
        P.memset("dve", mid, 0.0)
        for it in range(BIS_N):
            step = BIS_R / (2 ** (it + 1))
            P.ts("dve", bjunk[:, 0:Sj], score[:, 0:Sj], mid[:, 0:1], ALU.is_ge, 0.0, ALU.add, accum=cnt)
            P.ts("dve", t1, cnt, float(TOPK), ALU.is_ge, 2.0 * step, ALU.mult)
            P.ts("dve", mid, t1, -step, ALU.add, mid[:, 0:1], ALU.add)
        P.ts("dve", thr[:, j:j + 1], mid, -BIS_R / (2 ** BIS_N), ALU.add)

    def p2_mask(j):
        nblk = 8 * (j + 1)
        for sg in range(nblk // 4):
            m_ = mk[sg % 2]
            P.ts("dve", m_, score[:, sg * 512:(sg + 1) * 512], thr[:, j:j + 1], ALU.is_ge)
            tp = P.bank(5, F32, 512)
            for q in range(4):
                P.tr(tp[:, q * 128:(q + 1) * 128], m_[:, q * 128:(q + 1) * 128], ident_b)
            P.ts("dve", maskT[:, sg * 4:(sg + 1) * 4, :], tp.re("p (q t) -> p q t", q=4), 640.0, ALU.mult, -640.0, ALU.add)

    def p2_attn(j):
        nblk = 8 * (j + 1)
        P.dma(qT_b.re("p m t -> p (m t)"), qT_s[j])
        for cc in range(2):
            for hq in range(2):
                bk = P.bank(2 + cc * 2 + hq)
                for hh in range(4):
                    h = hq * 4 + hh
                    P.mm(bk[:, hh * 128:(hh + 1) * 128], wuk[:, h, cc * 128:(cc + 1) * 128], qT_b[:, h, :])
                P.copy("act", qa_b[:, cc, hq * 4:(hq + 1) * 4, :], bk.re("p (h t) -> p h t", h=4))
        for hq in range(2):
            OT = [P.bank(2 + 3 * hq), P.bank(3 + 3 * hq)]
            Z = P.bank(4 + 3 * hq)
            def emit_lg(i, hq=hq):
                cb = ckvb[cntr["cki"] % 4]
                cntr["cki"] += 1
                P.dma(cb, ckv_s[i])
                lg = P.bank(i % 2)
                for cc in range(2):
                    P.mm(lg, ckvT[:, cc, i * 128:(i + 1) * 128],
                         qa_b[:, cc, hq * 4:(hq + 1) * 4, :].re("p h t -> p (h t)"),
                         start=(cc == 0), stop=False)
                P.mm(lg.re("p (h t) -> p h t", h=4), ident_b,
                     maskT[:, i, :].re("p (o t) -> p o t", o=1).bc([128, 4, 128]), start=False, stop=True)
                PT = PTb[cntr["pti"] % 3]
                cntr["pti"] += 1
                P.act(PT.re("p h t -> p (h t)"), lg, AF.Exp, scale=SCALE)
                return cb, PT.re("p h t -> p (h t)")

            pend = emit_lg(0)
            for i in range(nblk):
                nxt = emit_lg(i + 1) if i + 1 < nblk else None
                cb, PTf = pend
                for cc in range(2):
                    P.mm(OT[cc], cb[:, cc * 128:(cc + 1) * 128], PTf, start=(i == 0), stop=(i == nblk - 1))
                P.mm(Z, ones128, PTf, start=(i == 0), stop=(i == nblk - 1))
                pend = nxt
        for hq in range(2):
            OT = [P.bank(2 + 3 * hq), P.bank(3 + 3 * hq)]
            Z = P.bank(4 + 3 * hq)
            P.recip(rZ2[hq], Z)
            for cc in range(2):
                P.tt("dve", OTn2[hq][:, cc, :, :].re("p h t -> p (h t)"), OT[cc], rZ2[hq], ALU.mult)
            ab = P.bank(hq)
            for hh in range(4):
                h = hq * 4 + hh
                for cc in range(2):
                    P.mm(ab[:, hh * 128:(hh + 1) * 128], wuv[:, h, cc, :], OTn2[hq][:, cc, hh, :],
                         start=(cc == 0), stop=(cc == 1))
            P.copy("act", aT[:, hq * 4:(hq + 1) * 4, j * 128:(j + 1) * 128], ab.re("p (h t) -> p h t", h=4))

    p2_idx(0)
    p2_bis(0)
    p2_mask(0)
    for j in range(LB):
        if j + 1 < LB:
            p2_idx(j + 1)
            p2_bis(j + 1)
        p2_attn(j)
        if j + 1 < LB:
            p2_mask(j + 1)
    if dbg:
        P.dma(dbg["aT"], aT)
        P.dma(dbg["thr"], thr)
    P.barrier()
    if T.get('stop') == 2:
        return

    at(36)
    mT = P.alloc("mT", (8, 1024), BF16)
    xTl = P.alloc("xTl", (KC, 1024), BF16)
    xst = [P.alloc("xst%d" % i, (KC, 128), F32) for i in range(2)]
    next_wq = rot([P.alloc("wq%d" % i, (KC, 512), BF16) for i in range(2)])
    gtmp = [dict((n, P.alloc("g%s%d" % (n, i), (512,), F32)) for n in ("x", "a", "b")) for i in range(2)]
    wsT = P.alloc("wsT", (8, 128), BF16)
    stgw = P.alloc("stgw", (8, 128), F32)
    bs_bc = P.alloc("bs_bc", (8, 128), F32)
    lng = P.alloc("lng", (1024,), F32)
    lnb = P.alloc("lnb", (1024,), F32)
    vtm = P.alloc("vtm", (LB, 1024), F32)
    vn = [P.alloc("vn%d" % i, (1024,), BF16) for i in range(2)]
    vj = P.alloc("vjunk", (1024,), F32)
    lst = [dict((n, P.alloc("l%s%d" % (n, i), (1,), F32)) for n in ("s1", "s2", "mu", "var", "sd", "rs"))
           for i in range(2)]
    ug = P.alloc("ug", (512,), F32)
    load_local_x(xTl, xst, False)
    gi = [0]

    def gelu_to(dst, src_ps, rs_operand, token_major, final_mul=None):
        g = gtmp[gi[0] % 2]
        gi[0] += 1
        if token_major:
            P.act(g["x"], src_ps, AF.Identity, scale=rs_operand)
        else:
            P.tt("dve", g["x"], src_ps, rs_operand, ALU.mult)
        P.act(g["a"], g["x"], AF.Square)
        P.ts("dve", g["a"], g["a"], 0.044715 * 1.5957691216, ALU.mult, 1.5957691216, ALU.add)
        P.tt("pool", g["b"], g["a"], g["x"], ALU.mult)
        P.act(g["a"], g["b"], AF.Sigmoid)
        if final_mul is None:
            P.tt("dve", dst, g["a"], g["x"], ALU.mult)
        else:
            P.tt("dve", g["b"], g["a"], g["x"], ALU.mult)
            P.tt("dve", dst, g["b"], final_mul, ALU.mult)

    P.dma(stgw, V(T["wsT_d"].b, T["wsT_d"].ap.rearrange("g s t -> s g t")))
    P.tt("pool", wsT, stgw, tri.re("p (o t) -> p o t", o=1).bc([128, 8, 128]), ALU.mult)
    P.dma(bs_bc.re("p g t -> p (g t)"), bcast(T["bs_d"]))
    P.dma(lng, bcast(T["lng_d"]))
    P.dma(lnb, bcast(T["lnb_d"]))
    for grp in range(2):
        wq = next_wq()
        load_w_cols(wq, C_UV + 1024 + grp * 512, C_UV + 1024 + (grp + 1) * 512, xst)
        for j in range(LB):
            bk = P.bank(bank_rr[0] % 4)
            bank_rr[0] += 1
            for k in range(KC):
                P.mm(bk, xTl[:, k, j * 128:(j + 1) * 128], wq[:, k, :], start=(k == 0), stop=(k == KC - 1))
            gelu_to(vtm[:, j, grp * 512:(grp + 1) * 512], bk, rs_tm[:, j:j + 1], True)
    for j in range(LB):
        i = j % 2
        l = lst[i]
        P.act(vj, vtm[:, j, :], AF.Identity, accum=l["s1"])
        P.act(vj, vtm[:, j, :], AF.Square, accum=l["s2"])
        P.ts("dve", l["mu"], l["s1"], 1.0 / 1024, ALU.mult)
        P.ts("dve", l["var"], l["mu"], l["mu"], ALU.mult, -1.0, ALU.mult)
        P.stt(l["var"], l["s2"], 1.0 / 1024, l["var"], ALU.mult, ALU.add)
        P.act(l["sd"], l["var"], AF.Sqrt, bias=epsb)
        P.recip(l["rs"], l["sd"])
        P.ts("dve", vj, vtm[:, j, :], l["mu"], ALU.subtract, l["rs"], ALU.mult)
        P.tt("dve", vj, vj, lng, ALU.mult)
        P.tt("dve", vn[i], vj, lnb, ALU.add)
        for gq in range(2):
            bk = P.bank(4 + gq)
            for gg in range(4):
                g = gq * 4 + gg
                P.mm(bk[:, gg * 128:(gg + 1) * 128], vn[i][:, g * 128:(g + 1) * 128], wsT[:, g, :])
            P.tt("dve", mT[:, gq * 4:(gq + 1) * 4, j * 128:(j + 1) * 128], bk.re("p (g t) -> p g t", g=4),
                 bs_bc[:, gq * 4:(gq + 1) * 4, :], ALU.add)
    for grp in range(2):
        def evac(m, half, bk, grp=grp):
            dst = mT[:, grp * 4 + m, half * 512:(half + 1) * 512]
            gelu_to(dst, bk, rs_bc[:, half * 512:(half + 1) * 512], False, final_mul=dst)
        fm_proj(next_wq(), xTl, xst, C_UV + grp * 512, 512, evac)
    if dbg:
        P.dma(dbg["mT"], mT)
    P.barrier()
    if T.get('stop') == 3:
        return

    at(84)
    xst = [P.alloc("xst%d" % i, (KC, 128), F32) for i in range(2)]
    mergedT = P.alloc("mergedT", (KC, 1024), BF16)
    next_wq = rot([P.alloc("wq%d" % i, (KC, 512), BF16) for i in range(2)])
    next_wbr = rot([P.alloc("wbr%d" % i, (8, 512), BF16) for i in range(2)])
    sig = [P.alloc("sig%d" % i, (512,), F32) for i in range(2)]
    prodA = [P.alloc("prodA%d" % i, (512,), F32) for i in range(2)]
    si = [0]

    def load_br(dst, src, c0):
        for k0 in range(0, 8, 4):
            s_ = xst[(k0 // 4) % 2].re("p k t -> p (k t)").re("p (a b) -> p a b", a=4)
            P.dma(s_, src[:, k0:k0 + 4, c0:c0 + 512])
            P.copy(eng2(), dst[:, k0:k0 + 4, :], s_)

    for dg in range(4):
        for br in range(2):
            wq = next_wq()
            wbr = next_wbr()
            load_br(wbr, T["w_bra"] if br == 0 else T["w_brg"], dg * 512)
            load_w_cols(wq, C_G + br * 2048 + dg * 512, C_G + br * 2048 + (dg + 1) * 512, xst)
            src = aT if br == 0 else mT
            for m in range(4):
                for half in range(2):
                    hs = slice(half * 512, (half + 1) * 512)
                    gb = P.bank(bank_rr[0] % 4)
                    bb = P.bank(4 + bank_rr[0] % 2)
                    bank_rr[0] += 1
                    for k in range(KC):
                        P.mm(gb, wq[:, k, m * 128:(m + 1) * 128], xTl[:, k, hs], start=(k == 0), stop=(k == KC - 1))
                    for k in range(8):
                        P.mm(bb, wbr[:, k, m * 128:(m + 1) * 128], src[:, k, hs], start=(k == 0), stop=(k == 7))
                    s_ = sig[si[0] % 2]
                    pa = prodA[si[0] % 2]
                    si[0] += 1
                    P.tt("dve", s_, gb, rs_bc[:, hs], ALU.mult)
                    P.act(s_, s_, AF.Sigmoid)
                    if br == 0:
                        P.tt("dve", mergedT[:, dg * 4 + m, hs], bb, s_, ALU.mult)
                    else:
                        P.tt("dve", pa, bb, s_, ALU.mult)
                        P.tt("dve", mergedT[:, dg * 4 + m, hs], mergedT[:, dg * 4 + m, hs], pa, ALU.add)
    P.barrier()
    if T.get('stop') == 4:
        return

    at(20)
    hacc = P.alloc("hacc", (LB, 2048), F32)
    xst = [P.alloc("xst%d" % i, (KC, 128), F32) for i in range(2)]
    at(132)
    wos = [P.alloc("wo%d" % i, (KC, 512), BF16) for i in range(2)]
    xl = [P.alloc("xl%d" % i, (512,), F32) for i in range(2)]
    for dg in range(4):
        wo = wos[dg % 2]
        for k0 in range(0, KC, 4):
            s_ = xst[(k0 // 4) % 2].re("p k t -> p (k t)").re("p (a b) -> p a b", a=4)
            P.dma(s_, T["w_out"][:, k0:k0 + 4, dg * 512:(dg + 1) * 512])
            P.copy(eng2(), wo[:, k0:k0 + 4, :], s_)
        for j in range(LB):
            bk = P.bank(bank_rr[0] % 4)
            bank_rr[0] += 1
            for k in range(KC):
                P.mm(bk, mergedT[:, k, j * 128:(j + 1) * 128], wo[:, k, :], start=(k == 0), stop=(k == KC - 1))
            x_ = xl[(dg * LB + j) % 2]
            P.dma(x_, x_loc[j][:, dg * 512:(dg + 1) * 512])
            P.tt("dve", hacc[:, j, dg * 512:(dg + 1) * 512], bk, x_, ALU.add)
    if dbg:
        P.dma(dbg["h"], hacc)
    P.barrier()
    if T.get('stop') == 5:
        return

    at(116)
    ohf = P.alloc("ohf", (LB, 8), F32)
    ohb = P.alloc("ohb", (LB, 8), BF16)
    posf = P.alloc("posf", (LB, 8), F32)
    combb = P.alloc("combb", (LB, 64), BF16)
    triS_b = P.alloc("triS_b", (128,), BF16)
    iota256 = P.alloc("iota256", (256,), F32)
    combS = P.alloc("combS", (2, 8), F32)
    assert P.top <= KB(120), P.top
    at(84)
    x2tm = P.alloc("x2tm", (LB, 2048), BF16)
    at(120)
    x2Tb = [P.alloc("x2Tb%d" % i, (KC, 128), BF16) for i in range(2)]
    wr = P.alloc("wr", (KC, 72), BF16)
    wrs = P.alloc("wrs", (KC, 72), F32)
    br_bc = P.alloc("br_bc", (72,), F32)
    rt = [dict((n, P.alloc("r%s%d" % (n, i), sh, F32)) for n, sh in
               (("s2", (1,)), ("sd", (1,)), ("rs", (1,)), ("lg", (72,)), ("gm", (1,)), ("ge", (8,)), ("gs", (1,)),
                ("gv", (1,)), ("el", (8, 8)), ("sel", (8,)), ("m1", (1,)), ("k1", (8,)), ("s2l", (8,)),
                ("m2", (1,)), ("k2", (8,)), ("dd", (1,)), ("q1", (1,)), ("q2", (1,)), ("wn", (8,)),
                ("ohv", (8,)))) for i in range(2)]
    hj = P.alloc("hjunk", (2048,), F32)
    trS_f = P.alloc("trS_f", (128,), F32)
    P.dma(wrs, T["wr_d"])
    P.copy("pool", wr, wrs)
    P.dma(br_bc, bcast(T["br_d"]))
    P.tt("pool", trS_f, tri, ident_f, ALU.subtract)
    P.copy("pool", triS_b, trS_f)
    P.iota(iota256, [[1, 256]], 0, 0)
    for j in range(LB):
        i = j % 2
        r = rt[i]
        oh = ohf[:, j, :]
        P.act(hj, hacc[:, j, :], AF.Square, accum=r["s2"])
        P.act(r["sd"], r["s2"], AF.Sqrt, scale=1.0 / D, bias=epsb)
        P.recip(r["rs"], r["sd"])
        P.act(x2tm[:, j, :], hacc[:, j, :], AF.Identity, scale=r["rs"])
        for kq in range(4):
            tp = P.bank(4 + (j * 4 + kq) % 2, F32, 512)
            for kk in range(4):
                k = kq * 4 + kk
                P.tr(tp[:, kk * 128:(kk + 1) * 128], x2tm[:, j, k * 128:(k + 1) * 128], ident_b)
            for kk in range(4):
                k = kq * 4 + kk
                P.act(x2Tb[i][:, k, :], tp[:, kk * 128:(kk + 1) * 128], AF.Identity, scale=g2[:, k:k + 1])
        lb = P.bank(j % 2)
        for k in range(KC):
            P.mm(lb[:, 0:72], x2Tb[i][:, k, :], wr[:, k, :], start=(k == 0), stop=(k == KC - 1))
        P.tt("dve", r["lg"], lb[:, 0:72], br_bc, ALU.add)
        P.reduce(r["gm"], r["lg"][:, 0:8], ALU.max)
        P.ts("dve", oh, r["lg"][:, 0:8], r["gm"], ALU.is_equal)
        P.copy("act", ohb[:, j, :], oh)
        P.ts("dve", r["ge"], r["lg"][:, 0:8], r["gm"], ALU.subtract)
        P.act(r["ge"], r["ge"], AF.Exp, accum=r["gs"])
        P.recip(r["gv"], r["gs"])
        P.tt("dve", r["el"], r["lg"][:, 8:72].re("p (g e) -> p g e", g=8),
             oh.re("p (g o) -> p g o", o=1).bc([128, 8, 8]), ALU.mult)
        P.reduce(r["sel"], r["el"].re("p g e -> p e g"), ALU.add)
        P.reduce(r["m1"], r["sel"], ALU.max)
        P.ts("dve", r["k1"], r["sel"], r["m1"], ALU.is_equal)
        P.stt(r["s2l"], r["k1"], NEG, r["sel"], ALU.mult, ALU.add)
        P.reduce(r["m2"], r["s2l"], ALU.max)
        P.ts("dve", r["k2"], r["s2l"], r["m2"], ALU.is_equal)
        P.tt("dve", r["dd"], r["m2"], r["m1"], ALU.subtract)
        P.act(r["dd"], r["dd"], AF.Exp)
        P.ts("dve", r["q1"], r["dd"], 1.0, ALU.add)
        P.recip(r["q1"], r["q1"])
        P.tt("dve", r["q2"], r["dd"], r["q1"], ALU.mult)
        P.ts("dve", r["wn"], r["k1"], r["q1"], ALU.mult)
        P.stt(r["wn"], r["k2"], r["q2"], r["wn"], ALU.mult, ALU.add)
        P.ts("dve", r["ohv"], oh, r["gv"], ALU.mult)
        for g in range(8):
            P.ts("dve", comb[:, j, g * 8:(g + 1) * 8], r["wn"], r["ohv"][:, g:g + 1], ALU.mult)
    P.copy("act", combb, comb)
    for j in range(LB):
        ps = P.bank(2 + j % 2)
        P.mm(ps[:, 0:8], triS_b, ohb[:, j, :], start=True, stop=(j == 0))
        for j2 in range(j):
            P.mm(ps[:, 0:8], ones128, ohb[:, j2, :], start=False, stop=(j2 == j - 1))
        P.copy("act", posf[:, j, :], ps[:, 0:8])
    if dbg:
        P.dma(dbg["comb"], comb)
    P.barrier()
    if T.get('stop') == 6:
        return

    CAP = 256
    at(120)
    wg_h = P.alloc("wg_h", (KC, 256), BF16)
    wu_h = P.alloc("wu_h", (KC, 256), BF16)
    wd_b = P.alloc("wd_b", (4, 2048), BF16)
    est = [P.alloc("est%d" % i, (8, 256), F32) for i in range(3)]
    xy = P.alloc("xy", (KC, CAP), BF16)
    x2gT = xy
    ygb = V(xy.b, xy.ap.rearrange("p k s -> p (k s)").rearrange("p (a b) -> p a b", a=2))
    yg = P.alloc("yg", (2, 2048), F32)
    hidT = P.alloc("hidT", (4, CAP), BF16)
    sgt = [P.alloc("sgt%d" % i, (CAP,), BF16) for i in range(2)]
    assert P.top <= KB(207), P.top
    P.top = off_wuk
    Pg = P.alloc("Pg", (LB, CAP), BF16)
    PgT = P.alloc("PgT", (2, 1024), BF16)
    ei = 0
    cast_rot = ("act", "dve", "act", "dve")
    weg, weu, wed = T["weg"], T["weu"], T["wed"]
    for g in range(T.get("n_experts", NE) // 8):
        for j in range(LB):
            P.ts("dve", Pg[:, j, :], iota256, posf[:, j, g:g + 1], ALU.is_equal, ohf[:, j, g:g + 1], ALU.mult)
        for sb in range(2):
            for jq in range(2):
                tp = P.bank(6 + jq)
                for jj in range(4):
                    j = jq * 4 + jj
                    P.tr(tp[:, jj * 128:(jj + 1) * 128], Pg[:, j, sb * 128:(sb + 1) * 128], ident_b)
                P.copy("act", PgT[:, sb, jq * 512:(jq + 1) * 512], tp)
        for sb in range(2):
            cp = P.bank(4 + sb)
            for j in range(LB):
                P.mm(cp[:, 0:8], Pg[:, j, sb * 128:(sb + 1) * 128], combb[:, j, g * 8:(g + 1) * 8],
                     start=(j == 0), stop=(j == LB - 1))
            P.copy("act", combS[:, sb, :], cp[:, 0:8])
        for k in range(KC):
            xp = P.bank(k % 4)
            for j in range(LB):
                P.mm(xp[:, 0:CAP], x2tm[:, j, k * 128:(k + 1) * 128], Pg[:, j, :], start=(j == 0), stop=(j == LB - 1))
            P.act(x2gT[:, k, :], xp[:, 0:CAP], AF.Identity, scale=g2[:, k:k + 1])
        for el in range(8):
            e = g * 8 + el
            for half in range(2):
                for (dst, srcw) in ((wg_h, weg), (wu_h, weu)):
                    for k0 in (0, 8):
                        s_ = est[ei % 3]
                        ce = cast_rot[ei % 4]
                        ei += 1
                        P.dma(s_, srcw[e][half][:, k0:k0 + 8, :])
                        P.copy(ce, dst[:, k0:k0 + 8, :], s_)
                for fcl in range(2):
                    fc = half * 2 + fcl
                    gb = P.bank(fcl)
                    ub = P.bank(2 + fcl)
                    for k in range(KC):
                        P.mm(gb[:, 0:CAP], wg_h[:, k, fcl * 128:(fcl + 1) * 128], x2gT[:, k, :],
                             start=(k == 0), stop=(k == KC - 1))
                    for k in range(KC):
                        P.mm(ub[:, 0:CAP], wu_h[:, k, fcl * 128:(fcl + 1) * 128], x2gT[:, k, :],
                             start=(k == 0), stop=(k == KC - 1))
                    sg_ = sgt[fc % 2]
                    P.act(sg_, gb[:, 0:CAP], AF.Silu)
                    P.tt("dve", hidT[:, fc, :], ub[:, 0:CAP], sg_, ALU.mult)
            for kf in range(4):
                s_ = est[ei % 3].re("p a b -> p (a b)")
                ce = cast_rot[ei % 4]
                ei += 1
                P.dma(s_, wed[e][:, kf, :])
                P.copy(ce, wd_b[:, kf, :], s_)
            for sb in range(2):
                for dq in range(4):
                    yb = P.bank(4 + dq % 2)
                    for kf in range(4):
                        P.mm(yb, hidT[:, kf, sb * 128:(sb + 1) * 128], wd_b[:, kf, dq * 512:(dq + 1) * 512],
                             start=(kf == 0), stop=(kf == 3))
                    ysl = yg[:, sb, dq * 512:(dq + 1) * 512]
                    if el == 0:
                        P.ts("dve", ysl, yb, combS[:, sb, el:el + 1], ALU.mult)
                    else:
                        P.stt(ysl, yb, combS[:, sb, el:el + 1], ysl, ALU.mult, ALU.add)
        P.copy("act", ygb, yg)
        for j in range(LB):
            for dq in range(4):
                ps = P.bank(6 + dq % 2)
                for sb in range(2):
                    P.mm(ps, PgT[:, sb, j * 128:(j + 1) * 128], ygb[:, sb, dq * 512:(dq + 1) * 512],
                         start=(sb == 0), stop=(sb == 1))
                hsl = hacc[:, j, dq * 512:(dq + 1) * 512]
                P.tt("dve", hsl, ps, hsl, ALU.add)
    P.barrier()
    if T.get('stop') == 7:
        return

    at(116)
    gf_bc = P.alloc("gf_bc", (2048,), F32)
    fj = P.alloc("fjunk", (2048,), F32)
    fo = [P.alloc("fo%d" % i, (2048,), F32) for i in range(2)]
    fs = [dict((n, P.alloc("f%s%d" % (n, i), (1,), F32)) for n in ("s2", "sd", "rs")) for i in range(2)]
    P.dma(gf_bc, bcast(T["gf_d"]))
    for j in range(LB):
        i = j % 2
        f = fs[i]
        P.act(fj, hacc[:, j, :], AF.Square, accum=f["s2"])
        P.act(f["sd"], f["s2"], AF.Sqrt, scale=1.0 / D, bias=epsb)
        P.recip(f["rs"], f["sd"])
        P.stt(fo[i], hacc[:, j, :], f["rs"], gf_bc, ALU.mult, ALU.mult)
        P.dma(out_d[j], fo[i])
    P.barrier()
    if T.get('stop') == 8:
        return


_NC_CACHE = {}


def _layout_inputs(inp, n_experts=NE):
    f = lambda a: np.ascontiguousarray(np.asarray(a, dtype=np.float32))
    x = f(inp["x"]).reshape(S, D)
    xT = np.ascontiguousarray(x.reshape(NB, 128, KC, 128).transpose(0, 3, 2, 1)).reshape(NB, 128, 2048)
    xb = x.reshape(NB, 128, D)
    pk = lambda v: np.ascontiguousarray(f(v).reshape(-1, 128).T)
    pkm = lambda w: np.ascontiguousarray(f(w).reshape(-1, 128, w.shape[-1]).transpose(1, 0, 2))
    shared = {
        "xT_all": xT,
        "g1": pk(inp["norm1_g"]),
        "w_in": pkm(inp["w_in"]),
        "gkv": f(inp["kv_norm_g"]),
        "w_uk": f(inp["w_uk"]),
        "w_uv": f(inp["w_uv"]),
        "wsT": np.ascontiguousarray(f(inp["gmlp_ws"]).transpose(0, 2, 1)),
        "bs": f(inp["gmlp_bs"]).reshape(1024),
        "lng": f(inp["ln_v_g"]),
        "lnb": f(inp["ln_v_b"]),
        "w_bra": pkm(inp["w_br_attn"]),
        "w_brg": pkm(inp["w_br_gmlp"]),
        "w_out": pkm(inp["w_out"]),
        "g2": pk(inp["norm2_g"]),
        "wr": pkm(np.concatenate([f(inp["w_group"]), f(inp["w_router"])], axis=1)),
        "br": np.concatenate([f(inp["b_group"]), f(inp["b_router"])]),
        "weg": np.ascontiguousarray(f(inp["w_e_gate"][:n_experts]).reshape(n_experts, KC, 128, 2, 256).transpose(0, 3, 2, 1, 4)),
        "weu": np.ascontiguousarray(f(inp["w_e_up"][:n_experts]).reshape(n_experts, KC, 128, 2, 256).transpose(0, 3, 2, 1, 4)),
        "wed": np.ascontiguousarray(f(inp["w_e_down"][:n_experts]).reshape(n_experts, 4, 128, D).transpose(0, 2, 1, 3)),
        "gf": f(inp["norm_f_g"]),
    }
    maps = []
    for c in range(NCORES):
        blks = [8 * j + c for j in range(LB)]
        m = dict(shared)
        m["xT_loc"] = np.ascontiguousarray(xT[blks])
        m["x_loc"] = np.ascontiguousarray(xb[blks])
        m["trel"] = (c * 128 + np.arange(128, dtype=np.float32)).reshape(128, 1)
        maps.append(m)
    return maps


def kernel(**inputs):
    if "nc" not in _NC_CACHE:
        _NC_CACHE["nc"] = build(False)
    nc = _NC_CACHE["nc"]
    maps = _layout_inputs(inputs)
    res = run_bass_kernel_spmd(nc, maps, core_ids=list(range(NCORES)))
    out = np.zeros((NB, 128, D), np.float32)
    for c in range(NCORES):
        o = np.asarray(res.results[c]["out"]).reshape(LB, 128, D)
        for j in range(LB):
            out[8 * j + c] = o[j]
    return out.reshape(1, S, D)
```
